# Optimizing a Trainium2 kernel written in Bass

```python
import jax, jax.numpy as jnp
from jax import lax
import numpy as np

D_MODEL = 1024
BATCH = 8
SEQ = 4096
DEPTH = 2

N_A = DEPTH // 2
N_B = DEPTH - N_A
CHUNK = 128
GMLP_WIDTH = 2 * D_MODEL
GMLP_GROUPS = 8
GMLP_GROUP_DIM = GMLP_WIDTH // GMLP_GROUPS
HEAD_DIM = 64
N_HEADS = D_MODEL // HEAD_DIM
Q_BLOCK = 128
N_EXPERTS = 32
TOP_K = 4
D_EXPERT = D_MODEL
SWIGLU_LIMIT = 7.0
SWIGLU_ALPHA = 1.702
EXPERT_BLOCK = 256
EPS = 1e-6
NEG_INF = -1e30

kernel_name = "yoco_gmlp_fox_moe_adaln"


def rms_norm(x, g):
    xf = x.astype(jnp.float32)
    y = xf * lax.rsqrt(jnp.mean(xf * xf, axis=-1, keepdims=True) + EPS)
    return (y * g.astype(jnp.float32)).astype(x.dtype)


def layer_norm(x, g, b):
    xf = x.astype(jnp.float32)
    mu = jnp.mean(xf, axis=-1, keepdims=True)
    var = jnp.mean(jnp.square(xf - mu), axis=-1, keepdims=True)
    y = (xf - mu) * lax.rsqrt(var + EPS)
    return (y * g.astype(jnp.float32) + b.astype(jnp.float32)).astype(x.dtype)


def modulate(h, shift, scale):
    return h * (1 + scale) + shift


def ada_chunks(c, w, b, n):
    mod = jax.nn.silu(c) @ w + b
    return [m[:, None, :] for m in jnp.split(mod, n, axis=-1)]


def gmlp_mixer(h, w_in, v_g, v_b, w_s, b_s, w_out):
    bsz, s, _ = h.shape
    z = jax.nn.gelu(h @ w_in, approximate=False)
    u, v = jnp.split(z, 2, axis=-1)
    v = layer_norm(v, v_g, v_b)
    v = v.reshape(bsz, s // CHUNK, CHUNK, GMLP_GROUPS, GMLP_GROUP_DIM)
    causal = jnp.tril(jnp.ones((CHUNK, CHUNK), dtype=bool))
    w_causal = jnp.where(causal[None], w_s, 0).astype(v.dtype)
    mixed = jnp.einsum('gts,bnsgd->bntgd', w_causal, v) + b_s.T[:, :, None]
    gated = u * mixed.reshape(bsz, s, GMLP_WIDTH)
    return gated @ w_out


def shared_kv(x, c, kv_ada_w, kv_ada_b, kv_norm_g, kv_w, k_norm_g, fgate_b):
    bsz, s, _ = x.shape
    shift, scale = ada_chunks(c, kv_ada_w, kv_ada_b, 2)
    h = modulate(rms_norm(x, kv_norm_g), shift, scale)
    proj = h @ kv_w
    k = proj[..., :D_MODEL].reshape(bsz, s, N_HEADS, HEAD_DIM)
    v = proj[..., D_MODEL:2 * D_MODEL].reshape(bsz, s, N_HEADS, HEAD_DIM)
    k = rms_norm(k, k_norm_g)
    log_f = jax.nn.log_sigmoid((proj[..., 2 * D_MODEL:] + fgate_b).astype(jnp.float32))
    log_f_cum = jnp.cumsum(log_f, axis=1)
    return k, v, log_f_cum


def forgetting_attention(q, k, v, log_f_cum):
    bsz, s, nh, dh = q.shape
    scale = dh ** -0.5
    lf = jnp.transpose(log_f_cum, (0, 2, 1))
    outs = []
    for i in range(s // Q_BLOCK):
        q0, end = i * Q_BLOCK, (i + 1) * Q_BLOCK
        qb, kb, vb = q[:, q0:end], k[:, :end], v[:, :end]
        logits = jnp.einsum('bqhd,bkhd->bhqk', qb, kb).astype(jnp.float32) * scale
        logits = logits + (lf[:, :, q0:end, None] - lf[:, :, None, :end])
        q_pos = q0 + jnp.arange(Q_BLOCK)
        k_pos = jnp.arange(end)
        logits = jnp.where(k_pos[None, :] <= q_pos[:, None], logits, NEG_INF)
        p = jax.nn.softmax(logits, axis=-1).astype(vb.dtype)
        outs.append(jnp.einsum('bhqk,bkhd->bqhd', p, vb))
    return jnp.concatenate(outs, axis=1)


def fox_mixer(h, k, v, log_f_cum, w_qg, q_norm_g, w_o):
    bsz, s, _ = h.shape
    qg = h @ w_qg
    q = rms_norm(qg[..., :D_MODEL].reshape(bsz, s, N_HEADS, HEAD_DIM), q_norm_g)
    o = forgetting_attention(q, k, v, log_f_cum).reshape(bsz, s, D_MODEL)
    o = o * jax.nn.sigmoid(qg[..., D_MODEL:])
    return o @ w_o


def moe(h, router_w, router_b, w1, b1, w2, b2):
    bsz, s, d = h.shape
    t = bsz * s
    tk = t * TOP_K
    xt = h.reshape(t, d)
    logits = (xt @ router_w + router_b).astype(jnp.float32)
    top_val, top_idx = lax.top_k(logits, TOP_K)
    gates = jax.nn.softmax(top_val, axis=-1)
    e_flat = top_idx.reshape(-1)
    tok_flat = jnp.arange(tk) // TOP_K
    order = jnp.argsort(e_flat)
    e_sorted = e_flat[order]
    counts = jnp.bincount(e_flat, length=N_EXPERTS)
    starts = jnp.cumsum(counts) - counts
    padded = (counts + EXPERT_BLOCK - 1) // EXPERT_BLOCK * EXPERT_BLOCK
    pends = jnp.cumsum(padded)
    pstarts = pends - padded
    dest_sorted = pstarts[e_sorted] + (jnp.arange(tk) - starts[e_sorted])
    dest = jnp.zeros((tk,), dtype=dest_sorted.dtype).at[order].set(dest_sorted)
    n_blocks = -(-(tk + N_EXPERTS * (EXPERT_BLOCK - 1)) // EXPERT_BLOCK)
    buf = jnp.zeros((n_blocks * EXPERT_BLOCK, d), dtype=xt.dtype).at[dest].set(xt[tok_flat])
    block_expert = jnp.minimum(
        jnp.searchsorted(pends, jnp.arange(n_blocks) * EXPERT_BLOCK, side='right'),
        N_EXPERTS - 1)

    def run_block(args):
        xb, e = args
        gu = xb @ w1[e] + b1[e]
        glu_in, lin = jnp.split(gu, 2, axis=-1)
        glu_in = jnp.minimum(glu_in, SWIGLU_LIMIT)
        lin = jnp.clip(lin, -SWIGLU_LIMIT, SWIGLU_LIMIT)
        act = (lin + 1) * (glu_in * jax.nn.sigmoid(SWIGLU_ALPHA * glu_in))
        return act @ w2[e] + b2[e]

    y_buf = lax.map(run_block, (buf.reshape(n_blocks, EXPERT_BLOCK, d), block_expert))
    y = y_buf.reshape(-1, d)[dest].reshape(t, TOP_K, d)
    out = jnp.einsum('tk,tkd->td', gates.astype(y.dtype), y)
    return out.reshape(bsz, s, d)


def setup_inputs(seed: int = 0) -> dict:
    key = jax.random.key(seed)
    ks = jax.random.split(key, 32)
    D = D_MODEL

    def nrm(k, shape, s):
        return jax.random.normal(k, shape, jnp.float32) * s

    return {
        "x": nrm(ks[0], (BATCH, SEQ, D), 1.0),
        "c": nrm(ks[1], (BATCH, D), 1.0),
        "ada_w": nrm(ks[2], (DEPTH, D, 6 * D), 0.5 * D ** -0.5),
        "ada_b": nrm(ks[3], (DEPTH, 6 * D), 0.02),
        "norm_mix_g": 1.0 + nrm(ks[4], (DEPTH, D), 0.05),
        "norm_ffn_g": 1.0 + nrm(ks[5], (DEPTH, D), 0.05),
        "gmlp_w_in": nrm(ks[6], (N_A, D, 2 * GMLP_WIDTH), D ** -0.5),
        "gmlp_v_g": 1.0 + nrm(ks[7], (N_A, GMLP_WIDTH), 0.05),
        "gmlp_v_b": nrm(ks[8], (N_A, GMLP_WIDTH), 0.02),
        "gmlp_w_s": nrm(ks[9], (N_A, GMLP_GROUPS, CHUNK, CHUNK), 0.5 * CHUNK ** -0.5),
        "gmlp_b_s": 1.0 + nrm(ks[10], (N_A, GMLP_GROUPS, CHUNK), 0.1),
        "gmlp_w_out": nrm(ks[11], (N_A, GMLP_WIDTH, D), GMLP_WIDTH ** -0.5),
        "kv_ada_w": nrm(ks[12], (D, 2 * D), 0.5 * D ** -0.5),
        "kv_ada_b": nrm(ks[13], (2 * D,), 0.02),
        "kv_norm_g": 1.0 + nrm(ks[14], (D,), 0.05),
        "kv_w": nrm(ks[15], (D, 2 * D + N_HEADS), D ** -0.5),
        "k_norm_g": 1.0 + nrm(ks[16], (HEAD_DIM,), 0.05),
        "fgate_b": 2.0 + nrm(ks[17], (N_HEADS,), 0.1),
        "fox_w_qg": nrm(ks[18], (N_B, D, 2 * D), D ** -0.5),
        "q_norm_g": 1.0 + nrm(ks[19], (N_B, HEAD_DIM), 0.05),
        "fox_w_o": nrm(ks[20], (N_B, D, D), D ** -0.5),
        "router_w": nrm(ks[21], (DEPTH, D, N_EXPERTS), D ** -0.5),
        "router_b": nrm(ks[22], (DEPTH, N_EXPERTS), 0.01),
        "exp_w1": nrm(ks[23], (DEPTH, N_EXPERTS, D, 2 * D_EXPERT), D ** -0.5),
        "exp_b1": nrm(ks[24], (DEPTH, N_EXPERTS, 2 * D_EXPERT), 0.01),
        "exp_w2": nrm(ks[25], (DEPTH, N_EXPERTS, D_EXPERT, D), D_EXPERT ** -0.5),
        "exp_b2": nrm(ks[26], (DEPTH, N_EXPERTS, D), 0.01),
        "final_g": 1.0 + nrm(ks[27], (D,), 0.05),
    }


def reference(x, c, ada_w, ada_b, norm_mix_g, norm_ffn_g, gmlp_w_in, gmlp_v_g, gmlp_v_b,
              gmlp_w_s, gmlp_b_s, gmlp_w_out, kv_ada_w, kv_ada_b, kv_norm_g, kv_w, k_norm_g,
              fgate_b, fox_w_qg, q_norm_g, fox_w_o, router_w, router_b, exp_w1, exp_b1,
              exp_w2, exp_b2, final_g):
    k_sh = v_sh = lf_sh = None
    for l in range(DEPTH):
        sh_m, sc_m, gt_m, sh_f, sc_f, gt_f = ada_chunks(c, ada_w[l], ada_b[l], 6)
        if l == N_A:
            k_sh, v_sh, lf_sh = shared_kv(x, c, kv_ada_w, kv_ada_b, kv_norm_g, kv_w,
                                          k_norm_g, fgate_b)
        h = modulate(rms_norm(x, norm_mix_g[l]), sh_m, sc_m)
        if l < N_A:
            y = gmlp_mixer(h, gmlp_w_in[l], gmlp_v_g[l], gmlp_v_b[l], gmlp_w_s[l],
                           gmlp_b_s[l], gmlp_w_out[l])
        else:
            j = l - N_A
            y = fox_mixer(h, k_sh, v_sh, lf_sh, fox_w_qg[j], q_norm_g[j], fox_w_o[j])
        x = x + gt_m * y
        h = modulate(rms_norm(x, norm_ffn_g[l]), sh_f, sc_f)
        x = x + gt_f * moe(h, router_w[l], router_b[l], exp_w1[l], exp_b1[l],
                           exp_w2[l], exp_b2[l])
    return rms_norm(x, final_g)
```

```python
import contextlib
import os
import numpy as np
import ml_dtypes
import concourse.bass as bass
import concourse.mybir as mybir
from concourse.bass_utils import run_bass_kernel_spmd


F32 = mybir.dt.float32
BF16 = mybir.dt.bfloat16
I32 = mybir.dt.int32
AF = mybir.ActivationFunctionType
ALU = mybir.AluOpType
AX = mybir.AxisListType


class Tk:
    __slots__ = ("ap", "w", "r", "g", "name")

    def __init__(self, ap, name=""):
        self.ap = ap
        self.w = {}
        self.r = {}
        self.g = {}
        self.name = name

    def __getitem__(self, key):
        return self.ap[key]


class K:
    NDS = {"sp": 30, "act": 22, "pool": 44}

    def __init__(self, nc, stack):
        self.nc = nc
        self.stack = stack
        self.eng = dict(pe=nc.tensor, act=nc.scalar, dve=nc.vector, pool=nc.gpsimd, sp=nc.sync)
        self.sems = {}
        for e in self.eng:
            self.sems[("c", e)] = stack.enter_context(nc.semaphore("c_" + e))
        for e in ("sp", "act", "pool"):
            for i in range(self.NDS[e]):
                self.sems[("d", e, i)] = stack.enter_context(nc.semaphore("d_%s%d" % (e, i)))
        self.cnt = {s: 0 for s in self.sems}
        self.dn = {e: 0 for e in ("sp", "act", "pool")}
        self.waited = {}
        self.uid = 0
        self.ninst = 0

    def sb(self, shape, dtype, stack=None, name=None):
        self.uid += 1
        nm = "%s_%d" % (name or "t", self.uid)
        t = (stack or self.stack).enter_context(self.nc.sbuf_tensor(nm, list(shape), dtype))
        return Tk(t, nm)

    def ps(self, shape, dtype, stack=None, name=None):
        self.uid += 1
        nm = "%s_%d" % (name or "p", self.uid)
        t = (stack or self.stack).enter_context(self.nc.psum_tensor(nm, list(shape), dtype))
        return Tk(t, nm)

    def dram(self, name, shape, dtype, kind="Internal"):
        t = self.nc.dram_tensor(name, list(shape), dtype, kind=kind).ap()
        return Tk(t, name)

    def op(self, eng, fn, reads=(), writes=(), dma=False, join=False):
        deps = {}

        def add(s, v):
            if deps.get(s, 0) < v:
                deps[s] = v

        for t in reads:
            for s, v in t.w.items():
                add(s, v)
        for t in writes:
            if join:
                for s, v in t.g.items():
                    add(s, v)
            else:
                t.g = dict(t.w)
                for s, v in t.r.items():
                    if t.g.get(s, 0) < v:
                        t.g[s] = v
                for s, v in t.w.items():
                    add(s, v)
            for s, v in t.r.items():
                add(s, v)
        e = self.eng[eng]
        for s, v in deps.items():
            if eng == "pe" and s == ("c", "pe"):
                continue
            if self.waited.get((eng, s), 0) >= v:
                continue
            e.wait_ge(self.sems[s], v)
            self.waited[(eng, s)] = v
            self.ninst += 1
        if dma:
            i = self.dn[eng]
            self.dn[eng] += 1
            skey = ("d", eng, i % self.NDS[eng])
            inc = 16
            prev = self.cnt[skey]
            if prev > 0 and self.waited.get((eng, skey), 0) < prev:
                e.wait_ge(self.sems[skey], prev)
                self.waited[(eng, skey)] = prev
                self.ninst += 1
        ins = fn(e)
        self.ninst += 1
        if dma:
            pass
        else:
            skey = ("c", eng)
            inc = 1
        ins.then_inc(self.sems[skey], inc)
        self.cnt[skey] += inc
        val = self.cnt[skey]
        for t in reads:
            if t.r.get(skey, 0) < val:
                t.r[skey] = val
        for t in writes:
            if join:
                if t.w.get(skey, 0) < val:
                    t.w[skey] = val
            else:
                t.w = {skey: val}
                t.r = {}
        return (skey, val)

    def barrier(self, engines=None):
        for eng in (engines or self.eng):
            e = self.eng[eng]
            for s, v in self.cnt.items():
                if v == 0 or self.waited.get((eng, s), 0) >= v:
                    continue
                e.wait_ge(self.sems[s], v)
                self.waited[(eng, s)] = v
                self.ninst += 1

    def dma(self, eng, out_ap, in_ap, reads=(), writes=(), join=False, **kw):
        return self.op(eng, lambda e: e.dma_start(out=out_ap, in_=in_ap, **kw), reads, writes, dma=True, join=join)

    def mm(self, out_t, out_ap, l_t, l_ap, r_t, r_ap, start=True, stop=True):
        return self.op("pe", lambda e: e.matmul(out_ap, l_ap, r_ap, start=start, stop=stop),
                       reads=[l_t, r_t], writes=[out_t])

    def tr(self, out_t, out_ap, in_t, in_ap, id_t, id_ap):
        return self.op("pe", lambda e: e.transpose(out_ap, in_ap, id_ap), reads=[in_t, id_t], writes=[out_t])


class Rot:
    def __init__(self, tiles):
        self.tiles = tiles
        self.i = 0

    def next(self):
        t = self.tiles[self.i % len(self.tiles)]
        self.i += 1
        return t


S = 4096
D = 1024
NT = 32
EPS = 1e-6
NCF = 900
NCB = 640 + 2048
C_ID, C_U, C_US, C_L, C_ONE, C_S127, C_IE, C_IP, C_IB = 0, 128, 256, 384, 512, 640, 768, 800, 801
B_ID, B_MA, B_ONE, B_NI, B_NM = 0, 128, 384, 512, 640


def make_consts():
    cf = np.zeros((128, NCF), np.float32)
    p = np.arange(128)[:, None]
    f = np.arange(128)[None, :]
    cf[:, C_ID:C_ID + 128] = (p == f)
    cf[:, C_U:C_U + 128] = (p <= f)
    cf[:, C_US:C_US + 128] = (p < f)
    cf[:, C_L:C_L + 128] = (f <= p)
    cf[:, C_ONE:C_ONE + 128] = 1.0
    cf[127, C_S127:C_S127 + 128] = 1.0
    cf[:, C_IE:C_IE + 32] = np.arange(32)[None, :]
    cf[:, C_IP] = np.arange(128)
    cf[:, C_IB:C_IB + 96] = np.arange(96)[None, :]
    cb = np.zeros((128, NCB), np.float32)
    cb[:, B_ID:B_ID + 128] = (p == f)
    f2 = np.arange(256)[None, :]
    cb[:, B_MA:B_MA + 256] = (p <= f2)
    cb[:, B_ONE:B_ONE + 128] = 1.0
    cb[:, B_NI:B_NI + 128] = -30000.0 * (p == f)
    f5 = np.arange(512)[None, :]
    for j in range(4):
        cb[:, B_NM + 512 * j:B_NM + 512 * (j + 1)] = (p + 128 * j > f5)
    return cf, cb.astype(ml_dtypes.bfloat16)


def declare_inputs(nc, only=None):
    def I(name, shape, dt=F32):
        if only is not None and name not in only:
            return None
        return nc.dram_tensor(name, list(shape), dt, kind="ExternalInput").ap()
    io = dict(
        x=I("x", [S, D]), c8=I("c8", [8, 128]), ada_w=I("ada_w", [2048, 6144]), ada_b=I("ada_b", [2, 6144]),
        nmg=I("nmg", [16, 128]), nfg=I("nfg", [16, 128]), gw_in=I("gw_in", [1024, 4096]), gvg=I("gvg", [16, 128]),
        gvb=I("gvb", [1, 2048]), gws=I("gws", [8, 128, 128]), gbs=I("gbs", [1, 1024]), gw_out=I("gw_out", [2048, 1024]),
        kvaw=I("kvaw", [1024, 2048]), kvab=I("kvab", [1, 2048]), kvng=I("kvng", [8, 128]), kvw=I("kvw", [1024, 2064]),
        kng=I("kng", [1, 64]), fgb=I("fgb", [1, 16]), wqg=I("wqg", [1024, 2048]), qng=I("qng", [1, 64]),
        wo=I("wo", [1024, 1024]), rw=I("rw", [2048, 32]), rb=I("rb", [2, 32]),
        w1=I("w1", [65536, 2048]), b1=I("b1", [64 * 128, 16]), w2=I("w2", [65536, 1024]), b2=I("b2", [64, 1024]),
        fing=I("fing", [1, 1024]), cf=I("cf", [128, NCF]), cb=I("cb", [128, NCB], BF16),
    )
    return io


class G:
    pass


def setup(k, io, g):
    nc = k.nc
    g.cf = k.sb([128, NCF], F32, name="cf")
    g.cb = k.sb([128, NCB], BF16, name="cb")
    k.dma("sp", g.cf[:, :], io["cf"][:, :], writes=[g.cf])
    k.dma("sp", g.cb[:, :], io["cb"][:, :], writes=[g.cb])
    g.psum = Rot([k.ps([128, 512], F32, name="pb%d" % i) for i in range(6)])
    g.psb = k.ps([128, 1024], BF16, name="pbb")
    g.psb2 = k.ps([128, 1024], BF16, name="pbb2")
    cf = g.cf
    g.bc_w = nc.gpsimd.to_reg(65535)
    g.bc_b = nc.gpsimd.to_reg(63)
    g.bc_c = nc.gpsimd.to_reg(64 * 128 - 1)
    rows = k.sb([64, 128], F32, name="rows")
    srcs = [(io["nmg"][0:8, :], 0), (io["nfg"][0:8, :], 8), (io["kvng"][:, :], 16), (io["nmg"][8:16, :], 24),
            (io["nfg"][8:16, :], 32), (io["c8"][:, :], 40), (io["gvg"][:, :], 48)]
    for ap, r0 in srcs:
        n = ap.shape[0]
        k.dma("sp", rows[r0:r0 + n, :], ap, writes=[rows])
    pt = g.psum.next()
    k.tr(pt, pt[:, 0:64], rows, rows[0:64, :], cf, cf[0:64, C_ID:C_ID + 64])
    g.cols = k.sb([128, 64], F32, name="cols")
    k.op("dve", lambda e: e.tensor_copy(out=g.cols[:, :], in_=pt[:, 0:64]), reads=[pt], writes=[g.cols])
    silu = k.sb([128, 8], F32, name="silu")
    k.op("act", lambda e: e.activation(out=silu[:, :], in_=g.cols[:, 40:48], func=AF.Silu), reads=[g.cols], writes=[silu])
    one11 = cf[0:1, C_ONE:C_ONE + 1]
    ones_row = cf[0:1, C_ONE:C_ONE + 128]
    g.modT = [k.sb([128, 48], F32, name="modT%d" % l) for l in range(2)]
    g.modK = k.sb([128, 16], F32, name="modK")
    g.gates = [[k.sb([128, 1024], F32, name="gate%d%d" % (l, w)) for w in range(2)] for l in range(2)]
    with contextlib.ExitStack() as st:
        silu_rep = k.sb([128, 8, 128], F32, stack=st, name="silurep")
        k.op("dve", lambda e: e.tensor_copy(out=silu_rep[:, :, :], in_=silu[:, :].unsqueeze(2).to_broadcast([128, 8, 128])),
             reads=[silu], writes=[silu_rep])
        wch = Rot([k.sb([128, 8, 512], F32, stack=st, name="wch") for _ in range(2)])
        bch = Rot([k.sb([1, 512], F32, stack=st, name="bch") for _ in range(2)])
        groups = [(io["ada_w"][0:1024, :], io["ada_b"][0:1, :], 12, g.modT[0], 0),
                  (io["ada_w"][1024:2048, :], io["ada_b"][1:2, :], 12, g.modT[1], 1),
                  (io["kvaw"][:, :], io["kvab"][0:1, :], 4, g.modK, None)]
        for W, Bv, nch, dst, l in groups:
            pm = g.psum.next()
            for cc in range(nch):
                w = wch.next()
                b = bch.next()
                k.dma("sp" if cc % 2 == 0 else "act", w[:, :, :],
                      W[:, cc * 512:(cc + 1) * 512].rearrange("(k p) n -> p k n", p=128), writes=[w])
                k.dma("sp", b[:, :], Bv[:, cc * 512:(cc + 1) * 512], writes=[b])
                for q in range(4):
                    j = cc * 4 + q
                    for kc in range(8):
                        k.mm(pm, pm[:, j:j + 1], w, w[:, kc, q * 128:(q + 1) * 128], silu, silu[:, kc:kc + 1],
                             start=(kc == 0), stop=False)
                    k.mm(pm, pm[:, j:j + 1], b, b[0:1, q * 128:(q + 1) * 128], cf, one11, start=False, stop=True)
                if l is not None and cc in (4, 5, 10, 11):
                    pg = g.psum.next()
                    for kc in range(8):
                        k.mm(pg, pg[:, :], silu_rep, silu_rep[:, kc, :], w, w[:, kc, :], start=(kc == 0), stop=False)
                    k.mm(pg, pg[:, :], cf, ones_row, b, b[0:1, :], start=False, stop=True)
                    gt = g.gates[l][0 if cc < 6 else 1]
                    half = cc % 2
                    k.op("act", lambda e: e.copy(out=gt[:, half * 512:(half + 1) * 512], in_=pg[:, :]),
                         reads=[pg], writes=[gt])
            k.op("dve", lambda e: e.tensor_copy(out=dst[:, 0:nch * 4], in_=pm[:, 0:nch * 4]), reads=[pm], writes=[dst])
        k.barrier()
    g.A = k.sb([128, 5, 8], F32, name="A")
    specs = [(0, g.modT[0], 8, 0), (1, g.modT[0], 32, 8), (2, g.modK, 8, 16), (3, g.modT[1], 8, 24), (4, g.modT[1], 32, 32)]
    g.Bsh = []
    for n, mt, sc0, g0 in specs:
        k.op("dve", lambda e: e.scalar_tensor_tensor(out=g.A[:, n, :], in0=mt[:, sc0:sc0 + 8], scalar=1.0,
                                                     in1=g.cols[:, g0:g0 + 8], op0=ALU.add, op1=ALU.mult),
             reads=[mt, g.cols], writes=[g.A])
        g.Bsh.append((mt, sc0 - 8))
    return g


def rms_rstd(k, x_t, x_ap, junk, ss, rstd):
    k.op("act", lambda e: e.activation(out=junk[:, :], in_=x_ap, func=AF.Square, accum_out=ss[:, 0:1]),
         reads=[x_t], writes=[junk, ss])
    k.op("dve", lambda e: e.tensor_scalar(out=rstd[:, 0:1], in0=ss[:, 0:1], scalar1=1.0 / D, scalar2=EPS,
                                          op0=ALU.mult, op1=ALU.add), reads=[ss], writes=[rstd])
    k.op("act", lambda e: e.activation(out=rstd[:, 0:1], in_=rstd[:, 0:1], func=AF.Sqrt), reads=[rstd], writes=[rstd])
    k.op("dve", lambda e: e.reciprocal(out=rstd[:, 0:1], in_=rstd[:, 0:1]), reads=[rstd], writes=[rstd])


def gmlp_phase(k, io, g, X_in, X_out, dbg=None, after_loads=None):
    cf, cb = g.cf, g.cb
    with contextlib.ExitStack() as st:
        w_in = k.sb([128, 8, 4096], BF16, stack=st, name="w_in")
        w_out = k.sb([128, 16, 1024], BF16, stack=st, name="w_out")
        for kc in range(8):
            k.dma("pool", w_in[:, kc, :], io["gw_in"][kc * 128:(kc + 1) * 128, :], writes=[w_in])
        for kc in range(16):
            k.dma("pool", w_out[:, kc, :], io["gw_out"][kc * 128:(kc + 1) * 128, :], writes=[w_out])
        if after_loads is not None:
            after_loads()
        WcT = k.sb([128, 8, 128], BF16, stack=st, name="WcT")
        Cc = k.sb([128, 16, 128], F32, stack=st, name="Cc")
        st2 = contextlib.ExitStack()
        ws_nat = k.sb([128, 8, 128], F32, stack=st2, name="ws_nat")
        k.dma("sp", ws_nat[:, :, :], io["gws"].rearrange("g t s -> t g s"), writes=[ws_nat])
        ws_m = k.sb([128, 8, 128], BF16, stack=st2, name="ws_m")
        k.op("dve", lambda e: e.tensor_tensor(out=ws_m[:, :, :], in0=ws_nat[:, :, :],
                                              in1=cf[:, C_L:C_L + 128].unsqueeze(1).to_broadcast([128, 8, 128]),
                                              op=ALU.mult), reads=[ws_nat, cf], writes=[ws_m])
        for gi in range(8):
            k.tr(g.psb, g.psb[:, gi * 128:(gi + 1) * 128], ws_m, ws_m[:, gi, :], cb, cb[:, B_ID:B_ID + 128])
        k.op("dve", lambda e: e.tensor_copy(out=WcT[:, :, :], in_=g.psb[:, :].rearrange("p (g t) -> p g t", g=8)),
             reads=[g.psb], writes=[WcT])
        rs_row = k.sb([1, 1024], F32, stack=st2, name="rs_row")
        for hf in range(2):
            pr = g.psum.next()
            k.mm(pr, pr[0:1, :], cb, cb[:, B_ONE:B_ONE + 1], WcT, WcT[:, hf * 4:(hf + 1) * 4, :], start=True, stop=True)
            k.op("dve", lambda e: e.tensor_copy(out=rs_row[0:1, hf * 512:(hf + 1) * 512], in_=pr[0:1, :]),
                 reads=[pr], writes=[rs_row])
        bs_row = k.sb([1, 1024], F32, stack=st2, name="bs_row")
        vb_row = k.sb([1, 2048], F32, stack=st2, name="vb_row")
        k.dma("sp", bs_row[:, :], io["gbs"][:, :], writes=[bs_row])
        k.dma("sp", vb_row[:, :], io["gvb"][:, :], writes=[vb_row])
        ones_row = cf[0:1, C_ONE:C_ONE + 128]
        for fo4 in range(4):
            pc = g.psum.next()
            for q in range(4):
                fo = fo4 * 4 + q
                gi = fo // 2
                k.mm(pc, pc[:, q * 128:(q + 1) * 128], vb_row, vb_row[0:1, fo * 128:(fo + 1) * 128],
                     rs_row, rs_row[0:1, gi * 128:(gi + 1) * 128], start=True, stop=False)
                k.mm(pc, pc[:, q * 128:(q + 1) * 128], cf, ones_row, bs_row, bs_row[0:1, gi * 128:(gi + 1) * 128],
                     start=False, stop=True)
            k.op("dve", lambda e: e.tensor_copy(out=Cc[:, fo4 * 4:(fo4 + 1) * 4, :],
                                                in_=pc[:, :].rearrange("p (q t) -> p q t", q=4)),
                 reads=[pc], writes=[Cc])
        k.barrier()
        st2.close()
        gam = g.cols
        xt_r = Rot([k.sb([128, 1024], F32, stack=st, name="xt") for _ in range(4)])
        ss_r = Rot([k.sb([128, 2], F32, stack=st, name="ss") for _ in range(2)])
        xh_r = Rot([k.sb([128, 1024], BF16, stack=st, name="xh") for _ in range(2)])
        hT_r = Rot([k.sb([128, 8, 128], BF16, stack=st, name="hT") for _ in range(2)])
        v_r = Rot([k.sb([128, 2048], F32, stack=st, name="v") for _ in range(1)])
        st_r = Rot([k.sb([128, 32], F32, stack=st, name="st") for _ in range(2)])
        vh_r = Rot([k.sb([128, 2048], BF16, stack=st, name="vh") for _ in range(2)])
        uT_r = Rot([k.sb([128, 16, 128], F32, stack=st, name="uT") for _ in range(1)])
        tmp_r = Rot([k.sb([128, 4, 128], F32, stack=st, name="tmp") for _ in range(1)])
        gT_r = Rot([k.sb([128, 16, 128], BF16, stack=st, name="gT") for _ in range(2)])
        x1_r = Rot([k.sb([128, 1024], F32, stack=st, name="x1") for _ in range(2)])
        A0 = g.A
        Bt, Bc = g.Bsh[0]
        gate = g.gates[0][0]
        T = {}

        def stL(ti):
            d = T.setdefault(ti, {})
            xt = xt_r.next()
            k.dma("sp", xt[:, :], X_in.ap[ti * 128:(ti + 1) * 128, :], reads=[X_in], writes=[xt])
            d["xt"] = xt

        def stA(ti):
            d = T[ti]
            xt = d["xt"]
            ss = ss_r.next()
            xh = xh_r.next()
            rms_rstd(k, xt, xt[:, :], xh, ss, ss)
            k.op("act", lambda e: e.activation(out=xh[:, :], in_=xt[:, :], func=AF.Copy, scale=ss[:, 0:1]),
                 reads=[xt, ss], writes=[xh])
            for kc in range(8):
                k.tr(g.psb, g.psb[:, kc * 128:(kc + 1) * 128], xh, xh[:, kc * 128:(kc + 1) * 128], cb, cb[:, B_ID:B_ID + 128])
            hT = hT_r.next()
            for kc in range(8):
                eng = "dve" if kc % 2 == 0 else "pool"
                if eng == "pool":
                    eng = "dve"
                k.op(eng, lambda e: e.tensor_scalar(out=hT[:, kc, :], in0=g.psb[:, kc * 128:(kc + 1) * 128],
                                                    scalar1=A0[:, 0, kc:kc + 1], scalar2=Bt[:, Bc + kc:Bc + kc + 1],
                                                    op0=ALU.mult, op1=ALU.add), reads=[g.psb, A0, Bt], writes=[hT], join=(kc > 0))
            d.update(xt=xt, hT=hT)

        def stB(ti):
            d = T[ti]; hT = d['hT']
            v = v_r.next()
            for nb in range(4):
                pv = g.psum.next()
                for kc in range(8):
                    k.mm(pv, pv[:, :], hT, hT[:, kc, :], w_in, w_in[:, kc, 2048 + nb * 512:2048 + (nb + 1) * 512],
                         start=(kc == 0), stop=(kc == 7))
                k.op("act", lambda e: e.activation(out=v[:, nb * 512:(nb + 1) * 512], in_=pv[:, :], func=AF.Gelu),
                     reads=[pv], writes=[v], join=(nb > 0))
            stt = st_r.next()
            for nb in range(4):
                k.op("dve", lambda e: e.bn_stats(out=stt[:, nb * 6:(nb + 1) * 6], in_=v[:, nb * 512:(nb + 1) * 512]),
                     reads=[v], writes=[stt])
            k.op("dve", lambda e: e.bn_aggr(out=stt[:, 24:26], in_=stt[:, 0:24]), reads=[stt], writes=[stt])
            k.op("dve", lambda e: e.tensor_scalar(out=stt[:, 26:27], in0=stt[:, 25:26], scalar1=EPS, scalar2=None,
                                                  op0=ALU.add), reads=[stt], writes=[stt])
            k.op("act", lambda e: e.activation(out=stt[:, 26:27], in_=stt[:, 26:27], func=AF.Sqrt), reads=[stt], writes=[stt])
            k.op("dve", lambda e: e.reciprocal(out=stt[:, 26:27], in_=stt[:, 26:27]), reads=[stt], writes=[stt])
            vh = vh_r.next()
            k.op("dve", lambda e: e.tensor_scalar(out=vh[:, :], in0=v[:, :], scalar1=stt[:, 24:25], scalar2=stt[:, 26:27],
                                                  op0=ALU.subtract, op1=ALU.mult), reads=[v, stt], writes=[vh])
            uT = uT_r.next()
            for f4 in range(4):
                pu = g.psum.next()
                for q in range(4):
                    fo = f4 * 4 + q
                    for kc in range(8):
                        k.mm(pu, pu[:, q * 128:(q + 1) * 128], w_in, w_in[:, kc, fo * 128:(fo + 1) * 128], hT, hT[:, kc, :],
                             start=(kc == 0), stop=(kc == 7))
                k.op("act", lambda e: e.activation(out=uT[:, f4 * 4:(f4 + 1) * 4, :],
                                                   in_=pu[:, :].rearrange("p (q t) -> p q t", q=4), func=AF.Gelu),
                     reads=[pu], writes=[uT], join=(f4 > 0))
            d.update(vh=vh, uT=uT)

        def stC(ti):
            d = T[ti]; vh = d['vh']; uT = d['uT']
            gT = gT_r.next()
            for f4 in range(4):
                pm = g.psum.next()
                for q in range(4):
                    fo = f4 * 4 + q
                    k.mm(pm, pm[:, q * 128:(q + 1) * 128], vh, vh[:, fo * 128:(fo + 1) * 128], WcT, WcT[:, fo // 2, :],
                         start=True, stop=True)
                tmp = tmp_r.next()
                k.op("dve", lambda e: e.tensor_tensor(out=tmp[:, :, :], in0=pm[:, :].rearrange("p (q t) -> p q t", q=4),
                                                      in1=gam[:, 48 + f4 * 4:48 + (f4 + 1) * 4].unsqueeze(2).to_broadcast([128, 4, 128]),
                                                      op=ALU.mult), reads=[pm, gam], writes=[tmp])
                k.op("pool", lambda e: e.tensor_tensor(out=tmp[:, :, :], in0=tmp[:, :, :], in1=Cc[:, f4 * 4:(f4 + 1) * 4, :],
                                                       op=ALU.add), reads=[tmp, Cc], writes=[tmp])
                k.op("pool", lambda e: e.tensor_tensor(out=gT[:, f4 * 4:(f4 + 1) * 4, :], in0=tmp[:, :, :],
                                                       in1=uT[:, f4 * 4:(f4 + 1) * 4, :], op=ALU.mult),
                     reads=[tmp, uT], writes=[gT], join=(f4 > 0))
            d.update(gT=gT)

        def stD(ti):
            d = T[ti]; gT = d['gT']; xt = d['xt']
            x1 = x1_r.next()
            for nb in range(2):
                py = g.psum.next()
                for fo in range(16):
                    k.mm(py, py[:, :], gT, gT[:, fo, :], w_out, w_out[:, fo, nb * 512:(nb + 1) * 512],
                         start=(fo == 0), stop=(fo == 15))
                k.op("dve", lambda e: e.tensor_tensor(out=x1[:, nb * 512:(nb + 1) * 512], in0=py[:, :],
                                                      in1=gate[:, nb * 512:(nb + 1) * 512], op=ALU.mult),
                     reads=[py, gate], writes=[x1], join=(nb > 0))
            k.op("pool", lambda e: e.tensor_tensor(out=x1[:, :], in0=x1[:, :], in1=xt[:, :], op=ALU.add),
                 reads=[x1, xt], writes=[x1])
            k.dma("sp", X_out.ap[ti * 128:(ti + 1) * 128, :], x1[:, :], reads=[x1], writes=[])

            del T[ti]

        stL(0)
        stL(1)
        stA(0)
        for ti in range(NT):
            if ti + 2 < NT:
                stL(ti + 2)
            if ti + 1 < NT:
                stA(ti + 1)
            stB(ti)
            stC(ti)
            if ti >= 1:
                stD(ti - 1)
        stD(NT - 1)
        k.barrier()


NB = 95
CAP = 4096


def IOA(ap):
    return bass.IndirectOffsetOnAxis(ap=ap, axis=0)


def route_phase(k, io, g, l, X_in, BUF, R):
    cf, cb = g.cf, g.cb
    n = 1 if l == 0 else 4
    Bt, Bc = g.Bsh[n]
    with contextlib.ExitStack() as st:
        Wr = k.sb([128, 8, 32], F32, stack=st, name="Wr")
        Wr2 = k.sb([128, 8, 32], F32, stack=st, name="Wr2")
        rbr = k.sb([1, 32], F32, stack=st, name="rbr")
        br2 = k.sb([1, 32], F32, stack=st, name="br2")
        carry = k.sb([128, 32], F32, stack=st, name="carry")
        k.dma("sp", Wr[:, :, :], io["rw"][l * 1024:(l + 1) * 1024, :].rearrange("(k p) e -> p k e", p=128), writes=[Wr])
        k.dma("sp", rbr[:, :], io["rb"][l:l + 1, :], writes=[rbr])
        k.op("dve", lambda e: e.tensor_tensor(out=Wr2[:, :, :], in0=Wr[:, :, :],
                                              in1=g.A[:, n, :].unsqueeze(2).to_broadcast([128, 8, 32]), op=ALU.mult),
             reads=[Wr, g.A], writes=[Wr2])
        pb = g.psum.next()
        for kc in range(8):
            k.mm(pb, pb[0:1, 0:32], Bt, Bt[:, Bc + kc:Bc + kc + 1], Wr, Wr[:, kc, :], start=(kc == 0), stop=False)
        k.mm(pb, pb[0:1, 0:32], cf, cf[0:1, C_ONE:C_ONE + 1], rbr, rbr[0:1, :], start=False, stop=True)
        k.op("dve", lambda e: e.tensor_copy(out=br2[:, :], in_=pb[0:1, 0:32]), reads=[pb], writes=[br2])
        k.op("dve", lambda e: e.memset(carry[:, :], 0.0), writes=[carry])
        xt_r = Rot([k.sb([128, 1024], F32, stack=st, name="rxt") for _ in range(4)])
        junk = k.sb([128, 1024], BF16, stack=st, name="rjunk")
        ss_r = Rot([k.sb([128, 2], F32, stack=st, name="rss") for _ in range(3)])
        xh_r = Rot([k.sb([128, 1024], F32, stack=st, name="rxh") for _ in range(3)])
        xb_r = Rot([k.sb([128, 1024], BF16, stack=st, name="rxb") for _ in range(4)])
        xT_r = Rot([k.sb([128, 8, 128], F32, stack=st, name="rxT") for _ in range(3)])
        sm_r = Rot([k.sb([128, 256], F32, stack=st, name="rsm") for _ in range(3)])
        oh_r = Rot([k.sb([128, 4, 32], F32, stack=st, name="roh") for _ in range(4)])
        di_r = Rot([k.sb([128, 4], I32, stack=st, name="rdi") for _ in range(3)])
        TT = {}

        RL = {}

        def rsl(ti):
            xt = xt_r.next()
            k.dma("sp", xt[:, :], X_in.ap[ti * 128:(ti + 1) * 128, :], reads=[X_in], writes=[xt])
            RL[ti] = xt

        def rs1(ti):
            xt = RL.pop(ti)
            ss = ss_r.next()
            rms_rstd(k, xt, xt[:, :], junk, ss, ss)
            xh = xh_r.next()
            xb = xb_r.next()
            k.op("act", lambda e: e.activation(out=xh[:, :], in_=xt[:, :], func=AF.Copy, scale=ss[:, 0:1]),
                 reads=[xt, ss], writes=[xh])
            k.op("dve", lambda e: e.tensor_copy(out=xb[:, :], in_=xh[:, :]), reads=[xh], writes=[xb])
            xT = xT_r.next()
            for hf in range(2):
                pt = g.psum.next()
                for q in range(4):
                    kc = hf * 4 + q
                    k.tr(pt, pt[:, q * 128:(q + 1) * 128], xh, xh[:, kc * 128:(kc + 1) * 128], cf, cf[:, C_ID:C_ID + 128])
                k.op("act" if hf == 0 else "dve",
                     (lambda e: e.copy(out=xT[:, 0:4, :], in_=pt[:, :].rearrange("p (q t) -> p q t", q=4))) if hf == 0 else
                     (lambda e: e.tensor_copy(out=xT[:, 4:8, :], in_=pt[:, :].rearrange("p (q t) -> p q t", q=4))),
                     reads=[pt], writes=[xT], join=(hf > 0))
            pl = g.psum.next()
            for kc in range(8):
                k.mm(pl, pl[:, 0:32], xT, xT[:, kc, :], Wr2, Wr2[:, kc, :], start=(kc == 0), stop=False)
            k.mm(pl, pl[:, 0:32], cf, cf[0:1, C_ONE:C_ONE + 128], br2, br2[0:1, :], start=False, stop=True)
            sm = sm_r.next()
            LG, T8, NT0, EX, SU, RS, MK, RKF, SF, DF = (sm[:, 0:32], sm[:, 32:40], sm[:, 40:41], sm[:, 41:45], sm[:, 45:46],
                                                        sm[:, 46:47], sm[:, 64:96], sm[:, 96:128], sm[:, 128:160], sm[:, 160:164])
            k.op("dve", lambda e: e.tensor_copy(out=LG, in_=pl[:, 0:32]), reads=[pl], writes=[sm])
            TT[ti] = (sm, xb)

        def rs2(ti):
            sm, xb = TT.pop(ti)
            LG, T8, NT0, EX, SU, RS, MK, RKF, SF, DF = (sm[:, 0:32], sm[:, 32:40], sm[:, 40:41], sm[:, 41:45], sm[:, 45:46],
                                                        sm[:, 46:47], sm[:, 64:96], sm[:, 96:128], sm[:, 128:160], sm[:, 160:164])
            k.op("dve", lambda e: e.max(out=T8, in_=LG), reads=[sm], writes=[sm])
            k.op("dve", lambda e: e.tensor_scalar(out=NT0, in0=sm[:, 32:33], scalar1=-1.0, scalar2=None, op0=ALU.mult),
                 reads=[sm], writes=[sm])
            k.op("act", lambda e: e.activation(out=EX, in_=sm[:, 32:36], func=AF.Exp, bias=NT0, scale=1.0, accum_out=SU),
                 reads=[sm], writes=[sm])
            k.op("dve", lambda e: e.reciprocal(out=RS, in_=SU), reads=[sm], writes=[sm])
            k.op("dve", lambda e: e.tensor_scalar(out=R.GATE[:, ti, :], in0=EX, scalar1=RS, scalar2=None, op0=ALU.mult),
                 reads=[sm], writes=[R.GATE])
            k.op("dve", lambda e: e.tensor_scalar(out=MK, in0=LG, scalar1=sm[:, 35:36], scalar2=None, op0=ALU.is_ge),
                 reads=[sm], writes=[sm])
            oh = oh_r.next()
            for kk in range(4):
                k.op("dve", lambda e: e.tensor_scalar(out=oh[:, kk, :], in0=LG, scalar1=sm[:, 32 + kk:33 + kk], scalar2=None,
                                                      op0=ALU.is_equal), reads=[sm], writes=[oh])
            pr = g.psum.next()
            k.mm(pr, pr[:, 0:32], cf, cf[:, C_US:C_US + 128], sm, MK, start=True, stop=True)
            k.mm(pr, pr[:, 32:64], cf, cf[:, C_ONE:C_ONE + 128], sm, MK, start=True, stop=True)
            k.op("dve", lambda e: e.tensor_tensor(out=RKF, in0=pr[:, 0:32], in1=carry[:, :], op=ALU.add),
                 reads=[pr, carry], writes=[sm])
            k.op("dve", lambda e: e.tensor_tensor(out=carry[:, :], in0=pr[:, 32:64], in1=carry[:, :], op=ALU.add),
                 reads=[pr, carry], writes=[carry])
            k.op("dve", lambda e: e.scalar_tensor_tensor(out=SF, in0=cf[:, C_IE:C_IE + 32], scalar=float(CAP), in1=RKF,
                                                         op0=ALU.mult, op1=ALU.add), reads=[cf, sm], writes=[sm])
            tmp = oh_r.next()
            for src, dst_ap, dst_t in ((SF, DF, sm), (RKF, R.RK[:, ti, :], R.RK), (cf[:, C_IE:C_IE + 32], R.EK[:, ti, :], R.EK)):
                k.op("dve", lambda e: e.tensor_tensor(out=tmp[:, :, :], in0=oh[:, :, :],
                                                      in1=src.unsqueeze(1).to_broadcast([128, 4, 32]), op=ALU.mult),
                     reads=[oh, sm, cf], writes=[tmp])
                k.op("dve", lambda e: e.tensor_reduce(out=dst_ap, in_=tmp[:, :, :], axis=AX.X, op=ALU.add),
                     reads=[tmp], writes=[dst_t])
            di = di_r.next()
            k.op("dve", lambda e: e.tensor_copy(out=di[:, :], in_=DF), reads=[sm], writes=[di])
            for kk in range(4):
                k.op("pool", lambda e: e.indirect_dma_start(out=BUF.ap[:, :], out_offset=IOA(di[:, kk:kk + 1]),
                                                            in_=xb[:, :], in_offset=None),
                     reads=[xb, di], writes=[], dma=True)

        rsl(0)
        rsl(1)
        rs1(0)
        for ti in range(NT):
            if ti + 2 < NT:
                rsl(ti + 2)
            if ti + 1 < NT:
                rs1(ti + 1)
            rs2(ti)
        t1 = k.sb([128, 32], F32, stack=st, name="t1")
        nb256 = k.sb([128, 32], F32, stack=st, name="nb256")
        bend = k.sb([128, 32], F32, stack=st, name="bend")
        b256 = k.sb([128, NB], F32, stack=st, name="b256")
        k.op("dve", lambda e: e.tensor_scalar(out=b256[:, :], in0=cf[:, C_IB:C_IB + NB], scalar1=256.0, scalar2=None,
                                              op0=ALU.mult), reads=[cf], writes=[b256])
        cmp = k.sb([128, 32, 16], F32, stack=st, name="cmp")
        k.op("dve", lambda e: e.tensor_tensor(out=cmp[:, :, :], in0=carry[:, :].unsqueeze(2).to_broadcast([128, 32, 16]),
                                              in1=b256[:, 0:16].unsqueeze(1).to_broadcast([128, 32, 16]), op=ALU.is_gt),
             reads=[carry, b256], writes=[cmp])
        k.op("dve", lambda e: e.tensor_reduce(out=t1[:, :], in_=cmp[:, :, :], axis=AX.X, op=ALU.add), reads=[cmp], writes=[t1])
        k.op("dve", lambda e: e.tensor_scalar(out=nb256[:, :], in0=t1[:, :], scalar1=256.0, scalar2=None, op0=ALU.mult),
             reads=[t1], writes=[nb256])
        k.op("dve", lambda e: e.tensor_copy(out=bend[:, 0:1], in_=nb256[:, 0:1]), reads=[nb256], writes=[bend])
        for e_ in range(1, 32):
            k.op("dve", lambda e: e.tensor_tensor(out=bend[:, e_:e_ + 1], in0=bend[:, e_ - 1:e_], in1=nb256[:, e_:e_ + 1],
                                                  op=ALU.add), reads=[bend, nb256], writes=[bend])
        k.op("dve", lambda e: e.tensor_tensor(out=R.BST[:, :], in0=bend[:, :], in1=nb256[:, :], op=ALU.subtract),
             reads=[bend, nb256], writes=[R.BST])
        big = k.sb([128, NB, 32], F32, stack=st, name="big")
        eb = k.sb([128, NB], F32, stack=st, name="eb")
        bst = k.sb([128, NB], F32, stack=st, name="bst")
        sbase = k.sb([128, NB], F32, stack=st, name="sbase")
        k.op("dve", lambda e: e.tensor_tensor(out=big[:, :, :], in0=bend[:, :].unsqueeze(1).to_broadcast([128, NB, 32]),
                                              in1=b256[:, :].unsqueeze(2).to_broadcast([128, NB, 32]), op=ALU.is_le),
             reads=[bend, b256], writes=[big])
        k.op("dve", lambda e: e.tensor_reduce(out=eb[:, :], in_=big[:, :, :], axis=AX.X, op=ALU.add), reads=[big], writes=[eb])
        k.op("dve", lambda e: e.tensor_tensor(out=big[:, :, :], in0=big[:, :, :],
                                              in1=bend[:, :].unsqueeze(1).to_broadcast([128, NB, 32]), op=ALU.mult),
             reads=[big, bend], writes=[big])
        k.op("dve", lambda e: e.tensor_reduce(out=bst[:, :], in_=big[:, :, :], axis=AX.X, op=ALU.max), reads=[big], writes=[bst])
        k.op("dve", lambda e: e.tensor_scalar(out=eb[:, :], in0=eb[:, :], scalar1=31.0, scalar2=None, op0=ALU.min),
             reads=[eb], writes=[eb])
        k.op("dve", lambda e: e.tensor_tensor(out=bst[:, :], in0=b256[:, :], in1=bst[:, :], op=ALU.subtract),
             reads=[b256, bst], writes=[bst])
        k.op("dve", lambda e: e.tensor_scalar(out=bst[:, :], in0=bst[:, :], scalar1=float(CAP - 256), scalar2=None, op0=ALU.min),
             reads=[bst], writes=[bst])
        k.op("dve", lambda e: e.scalar_tensor_tensor(out=sbase[:, :], in0=eb[:, :], scalar=float(CAP), in1=bst[:, :],
                                                     op0=ALU.mult, op1=ALU.add), reads=[eb, bst], writes=[sbase])
        tf = k.sb([128, NB, 8], F32, stack=st, name="tf")
        for h in range(2):
            k.op("dve", lambda e: e.tensor_scalar(out=tf[:, :, h], in0=sbase[:, :], scalar1=cf[:, C_IP:C_IP + 1],
                                                  scalar2=float(h * 128), op0=ALU.add, op1=ALU.add),
                 reads=[sbase, cf], writes=[tf])
        k.op("dve", lambda e: e.tensor_copy(out=R.IDT[:, :, :], in_=tf[:, :, 0:2]), reads=[tf], writes=[R.IDT])
        skp = k.sb([128, NB], F32, stack=st, name="skp")
        k.op("dve", lambda e: e.memset(skp[:, :], 0.0), writes=[skp])
        k.op("dve", lambda e: e.tensor_tensor(out=skp[:, 2:NB], in0=eb[:, 2:NB], in1=eb[:, 0:NB - 2], op=ALU.is_equal),
             reads=[eb, skp], writes=[skp])
        k.op("dve", lambda e: e.tensor_scalar(out=skp[:, :], in0=skp[:, :], scalar1=float(1 << 20), scalar2=None, op0=ALU.mult),
             reads=[skp], writes=[skp])
        for kc in range(8):
            k.op("dve", lambda e: e.tensor_scalar(out=tf[:, :, kc], in0=eb[:, :], scalar1=1024.0,
                                                  scalar2=float(l * 32 * 1024 + kc * 128), op0=ALU.mult, op1=ALU.add),
                 reads=[eb], writes=[tf])
        k.op("dve", lambda e: e.tensor_scalar(out=tf[:, :, :], in0=tf[:, :, :], scalar1=cf[:, C_IP:C_IP + 1], scalar2=None,
                                              op0=ALU.add), reads=[tf, cf], writes=[tf])
        k.op("dve", lambda e: e.tensor_tensor(out=tf[:, :, :], in0=tf[:, :, :], in1=skp[:, :].unsqueeze(2).to_broadcast([128, NB, 8]),
                                              op=ALU.add), reads=[tf, skp], writes=[tf])
        k.op("dve", lambda e: e.tensor_copy(out=R.IDW[:, :, :], in_=tf[:, :, :]), reads=[tf], writes=[R.IDW])
        k.op("dve", lambda e: e.scalar_tensor_tensor(out=tf[:, :, 0], in0=eb[:, :], scalar=float(l * 32), in1=skp[:, :],
                                                     op0=ALU.add, op1=ALU.add), reads=[eb, skp], writes=[tf])
        k.op("dve", lambda e: e.tensor_copy(out=R.IDB[:, :], in_=tf[:, :, 0]), reads=[tf], writes=[R.IDB])
        k.op("dve", lambda e: e.tensor_scalar(out=tf[:, :, 1], in0=tf[:, :, 0], scalar1=128.0, scalar2=cf[:, C_IP:C_IP + 1],
                                              op0=ALU.mult, op1=ALU.add), reads=[tf, cf], writes=[tf])
        k.op("dve", lambda e: e.tensor_copy(out=R.IDC[:, :], in_=tf[:, :, 1]), reads=[tf], writes=[R.IDC])
        ekf = R.EK[:, :, :].rearrange("p t k -> p (t k)")
        big2 = k.sb([128, 128, 32], F32, stack=st, name="big2")
        cif = k.sb([128, 128], F32, stack=st, name="cif")
        k.op("dve", lambda e: e.tensor_tensor(out=big2[:, :, :], in0=cf[:, C_IE:C_IE + 32].unsqueeze(1).to_broadcast([128, 128, 32]),
                                              in1=ekf.unsqueeze(2).to_broadcast([128, 128, 32]), op=ALU.is_equal),
             reads=[cf, R.EK], writes=[big2])
        k.op("dve", lambda e: e.tensor_tensor(out=big2[:, :, :], in0=big2[:, :, :],
                                              in1=R.BST[:, :].unsqueeze(1).to_broadcast([128, 128, 32]), op=ALU.mult),
             reads=[big2, R.BST], writes=[big2])
        k.op("dve", lambda e: e.tensor_reduce(out=cif[:, :], in_=big2[:, :, :], axis=AX.X, op=ALU.add), reads=[big2], writes=[cif])
        k.op("dve", lambda e: e.tensor_tensor(out=cif[:, :], in0=cif[:, :], in1=R.RK[:, :, :].rearrange("p t k -> p (t k)"),
                                              op=ALU.add), reads=[cif, R.RK], writes=[cif])
        k.op("dve", lambda e: e.tensor_copy(out=R.CIDX[:, :, :].rearrange("p t k -> p (t k)"), in_=cif[:, :]),
             reads=[cif], writes=[R.CIDX])
        k.barrier()


def cast_weights(k, io, l, W1B, W2B):
    for e_ in range(32):
        r0 = (l * 32 + e_) * 1024
        k.dma("pool", W1B.ap[e_ * 1024:(e_ + 1) * 1024, :], io["w1"][r0:r0 + 1024, :], writes=[W1B], join=(e_ > 0))
        k.dma("pool", W2B.ap[e_ * 1024:(e_ + 1) * 1024, :], io["w2"][r0:r0 + 1024, :], writes=[W2B], join=(e_ > 0))


class RState:
    def __init__(self, k):
        self.GATE = k.sb([128, NT, 4], F32, name="GATE")
        self.RK = k.sb([128, NT, 4], F32, name="RK")
        self.EK = k.sb([128, NT, 4], F32, name="EK")
        self.BST = k.sb([128, 32], F32, name="BST")
        self.IDT = k.sb([128, NB, 2], I32, name="IDT")
        self.IDW = k.sb([128, NB, 8], I32, name="IDW")
        self.IDB = k.sb([128, NB], I32, name="IDB")
        self.IDC = k.sb([128, NB], I32, name="IDC")
        self.CIDX = k.sb([128, NT, 4], I32, name="CIDX")


def expert_phase(k, io, g, l, BUF, Y, R, W1B, W2B, nblocks=NB):
    cf, cb = g.cf, g.cb
    n = 1 if l == 0 else 4
    Bt, Bc = g.Bsh[n]
    with contextlib.ExitStack() as st:
        w1_r = Rot([k.sb([128, 8, 2048], BF16, stack=st, name="w1e") for _ in range(2)])
        w2_r = Rot([k.sb([128, 8, 1024], BF16, stack=st, name="w2e") for _ in range(2)])
        b1_r = Rot([k.sb([128, 16], F32, stack=st, name="b1c") for _ in range(2)])
        b2_r = Rot([k.sb([2, 1024], BF16, stack=st, name="b2r") for _ in range(2)])
        xb_r = Rot([k.sb([128, 2, 1024], BF16, stack=st, name="exb") for _ in range(2)])
        xT_r = Rot([k.sb([128, 8, 256], BF16, stack=st, name="exT") for _ in range(2)])
        gl_r = Rot([k.sb([128, 256], F32, stack=st, name="gl") for _ in range(3)])
        sg_r = Rot([k.sb([128, 256], F32, stack=st, name="sg") for _ in range(3)])
        ll_r = Rot([k.sb([128, 256], F32, stack=st, name="ll") for _ in range(3)])
        aT_r = Rot([k.sb([128, 8, 256], BF16, stack=st, name="aT") for _ in range(2)])
        yo_r = Rot([k.sb([128, 2, 1024], F32, stack=st, name="yo") for _ in range(2)])
        psb_r = Rot([g.psb, g.psb2])
        ones_bf = cb[0:1, B_ONE:B_ONE + 128]
        blk = {}

        def load(b):
            d = dict(xb=xb_r.next(), w1e=w1_r.next(), w2e=w2_r.next(), b1r=b1_r.next(), b2r=b2_r.next())
            for h in range(2):
                k.op("pool", lambda e: e.indirect_dma_start(out=d["xb"][:, h, :], out_offset=None, in_=BUF.ap[:, :],
                                                            in_offset=IOA(R.IDT[:, b, h:h + 1])),
                     reads=[R.IDT], writes=[d["xb"]], dma=True, join=(h > 0))
            for kc in range(8):
                k.op("pool", lambda e: e.indirect_dma_start(out=d["w1e"][:, kc, :], out_offset=None, in_=W1B.ap[:, :],
                                                            in_offset=IOA(R.IDW[:, b, kc:kc + 1]), bounds_check=g.bc_w, oob_is_err=False),
                     reads=[R.IDW, W1B], writes=[d["w1e"]], dma=True, join=(kc > 0))
            k.op("pool", lambda e: e.indirect_dma_start(out=d["b1r"][:, :], out_offset=None, in_=io["b1"][:, :],
                                                        in_offset=IOA(R.IDC[:, b:b + 1]), bounds_check=g.bc_c, oob_is_err=False),
                 reads=[R.IDC], writes=[d["b1r"]], dma=True)
            for kc in range(8):
                k.op("pool", lambda e: e.indirect_dma_start(out=d["w2e"][:, kc, :], out_offset=None, in_=W2B.ap[:, :],
                                                            in_offset=IOA(R.IDW[:, b, kc:kc + 1]), bounds_check=g.bc_w, oob_is_err=False),
                     reads=[R.IDW, W2B], writes=[d["w2e"]], dma=True, join=(kc > 0))
            k.op("pool", lambda e: e.indirect_dma_start(out=d["b2r"][0:2, :], out_offset=None, in_=io["b2"][:, :],
                                                        in_offset=IOA(R.IDB[0:2, b:b + 1]), bounds_check=g.bc_b, oob_is_err=False),
                 reads=[R.IDB], writes=[d["b2r"]], dma=True)
            blk[b] = d

        def transp(b):
            d = blk[b]
            xb = d["xb"]
            xT = xT_r.next()
            d["xT"] = xT
            first = True
            for h in range(2):
                pt = psb_r.next()
                for kc in range(8):
                    k.tr(pt, pt[:, kc * 128:(kc + 1) * 128], xb, xb[:, h, kc * 128:(kc + 1) * 128], cb, cb[:, B_ID:B_ID + 128])
                for kc in range(8):
                    if h == 0:
                        k.op("dve", lambda e: e.tensor_scalar(out=xT[:, kc, h * 128:(h + 1) * 128], in0=pt[:, kc * 128:(kc + 1) * 128],
                                                              scalar1=g.A[:, n, kc:kc + 1], scalar2=Bt[:, Bc + kc:Bc + kc + 1],
                                                              op0=ALU.mult, op1=ALU.add), reads=[pt, g.A, Bt], writes=[xT],
                             join=not first)
                    else:
                        k.op("act", lambda e: e.activation(out=xT[:, kc, h * 128:(h + 1) * 128], in_=pt[:, kc * 128:(kc + 1) * 128],
                                                           func=AF.Identity, scale=g.A[:, n, kc:kc + 1],
                                                           bias=Bt[:, Bc + kc:Bc + kc + 1]), reads=[pt, g.A, Bt], writes=[xT],
                             join=not first)
                    first = False

        def h1(b):
            d = blk[b]
            w1e, b1c, xT = d["w1e"], d["b1r"], d["xT"]
            aT = aT_r.next()
            d["aT"] = aT
            for j2 in range(4):
                pg_ = g.psum.next()
                pl_ = g.psum.next()
                for q in range(2):
                    j = 2 * j2 + q
                    for bank, fo in ((pg_, j), (pl_, 8 + j)):
                        o = bank[:, q * 256:(q + 1) * 256]
                        for kc in range(8):
                            k.mm(bank, o, w1e, w1e[:, kc, fo * 128:(fo + 1) * 128], xT, xT[:, kc, :], start=(kc == 0), stop=(kc == 7))
                for q in range(2):
                    j = 2 * j2 + q
                    gl = gl_r.next()
                    sg = sg_r.next()
                    ll = ll_r.next()
                    k.op("dve", lambda e: e.tensor_scalar(out=gl[:, :], in0=pg_[:, q * 256:(q + 1) * 256], scalar1=b1c[:, j:j + 1],
                                                          scalar2=7.0, op0=ALU.add, op1=ALU.min), reads=[pg_, b1c], writes=[gl])
                    k.op("act", lambda e: e.activation(out=ll[:, :], in_=pl_[:, q * 256:(q + 1) * 256], func=AF.Identity,
                                                       bias=b1c[:, 8 + j:9 + j], scale=1.0), reads=[pl_, b1c], writes=[ll])
                    k.op("act", lambda e: e.activation(out=sg[:, :], in_=gl[:, :], func=AF.Sigmoid, scale=1.702),
                         reads=[gl], writes=[sg])
                    k.op("dve", lambda e: e.tensor_scalar(out=ll[:, :], in0=ll[:, :], scalar1=7.0, scalar2=-7.0,
                                                          op0=ALU.min, op1=ALU.max), reads=[ll], writes=[ll])
                    k.op("dve", lambda e: e.tensor_tensor(out=gl[:, :], in0=gl[:, :], in1=sg[:, :], op=ALU.mult),
                         reads=[gl, sg], writes=[gl])
                    k.op("dve", lambda e: e.scalar_tensor_tensor(out=aT[:, j, :], in0=ll[:, :], scalar=1.0, in1=gl[:, :],
                                                                 op0=ALU.add, op1=ALU.mult), reads=[ll, gl], writes=[aT], join=(j > 0))

        def yout(b):
            d = blk[b]
            aT, w2e, b2r = d["aT"], d["w2e"], d["b2r"]
            yo = yo_r.next()
            i = 0
            for h in range(2):
                for nb in range(2):
                    py = g.psum.next()
                    for fo in range(8):
                        k.mm(py, py[:, :], aT, aT[:, fo, h * 128:(h + 1) * 128], w2e, w2e[:, fo, nb * 512:(nb + 1) * 512],
                             start=(fo == 0), stop=False)
                    k.mm(py, py[:, :], cb, ones_bf, b2r, b2r[0:1, nb * 512:(nb + 1) * 512], start=False, stop=True)
                    k.op("act", lambda e: e.copy(out=yo[:, h, nb * 512:(nb + 1) * 512], in_=py[:, :]), reads=[py], writes=[yo],
                         join=(i > 0))
                    i += 1
            k.dma("sp", Y.ap[b * 256:(b + 1) * 256, :].rearrange("(h p) d -> p h d", p=128), yo[:, :, :], reads=[yo], writes=[])
            del blk[b]

        load(0)
        transp(0)
        for b in range(nblocks):
            if b + 1 < nblocks:
                load(b + 1)
            h1(b)
            if b + 1 < nblocks:
                transp(b + 1)
            yout(b)
        k.barrier()


def combine_phase(k, io, g, l, X_in, Y, X_out, R, final=False):
    cf = g.cf
    gate = g.gates[l][1]
    with contextlib.ExitStack() as st:
        xt_r = Rot([k.sb([128, 1024], F32, stack=st, name="cxt") for _ in range(4)])
        yk_r = Rot([k.sb([128, 4, 1024], F32, stack=st, name="cyk") for _ in range(4)])
        acc_r = Rot([k.sb([128, 1024], F32, stack=st, name="cacc") for _ in range(3)])
        if final:
            fg = k.sb([128, 1024], F32, stack=st, name="fg")
            k.dma("sp", fg[:, :], io["fing"][0:1, :].to_broadcast([128, 1024]), writes=[fg])
            junk = k.sb([128, 1024], BF16, stack=st, name="cjunk")
            ss_r = Rot([k.sb([128, 2], F32, stack=st, name="css") for _ in range(2)])
        CL = {}

        def cl(ti):
            xt = xt_r.next()
            k.dma("sp", xt[:, :], X_in.ap[ti * 128:(ti + 1) * 128, :], reads=[X_in], writes=[xt])
            yk = yk_r.next()
            for kk in range(4):
                k.op("pool", lambda e: e.indirect_dma_start(out=yk[:, kk, :], out_offset=None, in_=Y.ap[:, :],
                                                            in_offset=IOA(R.CIDX[:, ti, kk:kk + 1])),
                     reads=[R.CIDX], writes=[yk], dma=True, join=(kk > 0))
            CL[ti] = (xt, yk)

        cl(0)
        cl(1)
        for ti in range(NT):
            if ti + 2 < NT:
                cl(ti + 2)
            xt, yk = CL.pop(ti)
            acc = acc_r.next()
            k.op("dve", lambda e: e.tensor_scalar(out=acc[:, :], in0=yk[:, 0, :], scalar1=R.GATE[:, ti, 0:1], scalar2=None,
                                                  op0=ALU.mult), reads=[yk, R.GATE], writes=[acc])
            for kk in range(1, 4):
                k.op("dve", lambda e: e.scalar_tensor_tensor(out=acc[:, :], in0=yk[:, kk, :], scalar=R.GATE[:, ti, kk:kk + 1],
                                                             in1=acc[:, :], op0=ALU.mult, op1=ALU.add),
                     reads=[yk, R.GATE, acc], writes=[acc])
            k.op("dve", lambda e: e.tensor_tensor(out=acc[:, :], in0=acc[:, :], in1=gate[:, :], op=ALU.mult),
                 reads=[acc, gate], writes=[acc])
            k.op("dve", lambda e: e.tensor_tensor(out=acc[:, :], in0=acc[:, :], in1=xt[:, :], op=ALU.add),
                 reads=[acc, xt], writes=[acc])
            if final:
                ss = ss_r.next()
                rms_rstd(k, acc, acc[:, :], junk, ss, ss)
                k.op("dve", lambda e: e.scalar_tensor_tensor(out=acc[:, :], in0=acc[:, :], scalar=ss[:, 0:1], in1=fg[:, :],
                                                             op0=ALU.mult, op1=ALU.mult), reads=[acc, ss, fg], writes=[acc])
            k.dma("sp", X_out.ap[ti * 128:(ti + 1) * 128, :], acc[:, :], reads=[acc], writes=[])
        k.barrier()


NH = 16
DH = 64
LVL = 9


def head_rmsnorm(k, src_ps_list, dst_bf, wk, gb, st_tiles):
    raw, sq, hs = st_tiles
    for nb, ps in enumerate(src_ps_list):
        k.op("act", lambda e: e.copy(out=raw[:, nb * 512:(nb + 1) * 512], in_=ps[:, :]), reads=[ps], writes=[raw], join=(nb > 0))
    k.op("dve", lambda e: e.tensor_tensor(out=sq[:, :], in0=raw[:, :], in1=raw[:, :], op=ALU.mult), reads=[raw], writes=[sq])
    k.op("dve", lambda e: e.tensor_reduce(out=hs[:, 0:16], in_=sq[:, :].rearrange("p (h d) -> p h d", h=NH), axis=AX.X, op=ALU.add),
         reads=[sq], writes=[hs])
    k.op("dve", lambda e: e.tensor_scalar(out=hs[:, 0:16], in0=hs[:, 0:16], scalar1=1.0 / DH, scalar2=EPS, op0=ALU.mult, op1=ALU.add),
         reads=[hs], writes=[hs])
    k.op("act", lambda e: e.activation(out=hs[:, 0:16], in_=hs[:, 0:16], func=AF.Sqrt), reads=[hs], writes=[hs])
    k.op("dve", lambda e: e.reciprocal(out=hs[:, 0:16], in_=hs[:, 0:16]), reads=[hs], writes=[hs])
    k.op("dve", lambda e: e.tensor_tensor(out=sq[:, :].rearrange("p (h d) -> p h d", h=NH),
                                          in0=raw[:, :].rearrange("p (h d) -> p h d", h=NH),
                                          in1=hs[:, 0:16].unsqueeze(2).to_broadcast([128, NH, DH]), op=ALU.mult),
         reads=[raw, hs], writes=[sq])
    k.op("pool", lambda e: e.tensor_tensor(out=dst_bf[:, :].rearrange("p (h d) -> p h d", h=NH),
                                           in0=sq[:, :].rearrange("p (h d) -> p h d", h=NH),
                                           in1=gb[:, :].unsqueeze(1).to_broadcast([128, NH, DH]), op=ALU.mult),
         reads=[sq, gb], writes=[dst_bf])


def kvq_phase(k, io, g, X_in, KT, QT, GT, V, FP, FPT, after_loads=None):
    cf, cb = g.cf, g.cb
    with contextlib.ExitStack() as st:
        kvw = k.sb([128, 8, 2064], BF16, stack=st, name="kvw")
        wqg = k.sb([128, 8, 2048], BF16, stack=st, name="wqg")
        for kc in range(8):
            k.dma("pool", kvw[:, kc, :], io["kvw"][kc * 128:(kc + 1) * 128, :], writes=[kvw])
            k.dma("pool", wqg[:, kc, :], io["wqg"][kc * 128:(kc + 1) * 128, :], writes=[wqg])
        if after_loads is not None:
            after_loads()
        kgb = k.sb([128, 64], F32, stack=st, name="kgb")
        qgb = k.sb([128, 64], F32, stack=st, name="qgb")
        fgb = k.sb([128, 16], F32, stack=st, name="fgb")
        k.dma("sp", kgb[:, :], io["kng"][0:1, :].to_broadcast([128, 64]), writes=[kgb])
        k.dma("sp", qgb[:, :], io["qng"][0:1, :].to_broadcast([128, 64]), writes=[qgb])
        k.dma("sp", fgb[:, :], io["fgb"][0:1, :].to_broadcast([128, 16]), writes=[fgb])
        carry = k.sb([128, 16], F32, stack=st, name="fcarry")
        k.op("dve", lambda e: e.memset(carry[:, :], 0.0), writes=[carry])
        xt_r = Rot([k.sb([128, 1024], F32, stack=st, name="axt") for _ in range(4)])
        junk = k.sb([128, 1024], BF16, stack=st, name="ajunk")
        ss_r = Rot([k.sb([128, 2], F32, stack=st, name="ass") for _ in range(2)])
        xh_r = Rot([k.sb([128, 1024], BF16, stack=st, name="axh") for _ in range(2)])
        hk_r = Rot([k.sb([128, 8, 128], BF16, stack=st, name="hk") for _ in range(2)])
        hq_r = Rot([k.sb([128, 8, 128], BF16, stack=st, name="hq") for _ in range(2)])
        raw = k.sb([128, 1024], F32, stack=st, name="raw")
        sq = k.sb([128, 1024], F32, stack=st, name="sq")
        hs = k.sb([128, 64], F32, stack=st, name="hs")
        kn_r = Rot([k.sb([128, 1024], BF16, stack=st, name="kn") for _ in range(2)])
        vb_r = Rot([k.sb([128, 1024], BF16, stack=st, name="vb") for _ in range(2)])
        tT_r = Rot([k.sb([128, 8, 128], BF16, stack=st, name="tT") for _ in range(3)])
        gT_r = Rot([k.sb([128, 8, 128], BF16, stack=st, name="agT") for _ in range(2)])
        fpt_r = Rot([k.sb([16, 128], F32, stack=st, name="fpt") for _ in range(2)])
        fz_r = Rot([k.sb([128, 48], F32, stack=st, name="fz") for _ in range(2)])
        A = g.A
        Btk, Bck = g.Bsh[2]
        Btq, Bcq = g.Bsh[3]
        psb_r = Rot([g.psb, g.psb2])
        raw2 = k.sb([128, 1024], F32, stack=st, name="raw2")
        sq2 = k.sb([128, 1024], F32, stack=st, name="sq2")
        hs2 = k.sb([128, 64], F32, stack=st, name="hs2")
        T = {}

        XL = {}

        def stL(ti):
            xt = xt_r.next()
            k.dma("sp", xt[:, :], X_in.ap[ti * 128:(ti + 1) * 128, :], reads=[X_in], writes=[xt])
            XL[ti] = xt

        def stA(ti):
            xt = XL.pop(ti)
            ss = ss_r.next()
            rms_rstd(k, xt, xt[:, :], junk, ss, ss)
            xh = xh_r.next()
            k.op("act", lambda e: e.activation(out=xh[:, :], in_=xt[:, :], func=AF.Copy, scale=ss[:, 0:1]),
                 reads=[xt, ss], writes=[xh])
            pt = psb_r.next()
            for kc in range(8):
                k.tr(pt, pt[:, kc * 128:(kc + 1) * 128], xh, xh[:, kc * 128:(kc + 1) * 128], cb, cb[:, B_ID:B_ID + 128])
            hk = hk_r.next()
            hq = hq_r.next()
            for kc in range(8):
                k.op("dve", lambda e: e.tensor_scalar(out=hk[:, kc, :], in0=pt[:, kc * 128:(kc + 1) * 128],
                                                      scalar1=A[:, 2, kc:kc + 1], scalar2=Btk[:, Bck + kc:Bck + kc + 1],
                                                      op0=ALU.mult, op1=ALU.add), reads=[pt, A, Btk], writes=[hk], join=(kc > 0))
            for kc in range(8):
                k.op("act", lambda e: e.activation(out=hq[:, kc, :], in_=pt[:, kc * 128:(kc + 1) * 128], func=AF.Identity,
                                                   scale=A[:, 3, kc:kc + 1], bias=Btq[:, Bcq + kc:Bcq + kc + 1]),
                     reads=[pt, A, Btq, hk], writes=[hq], join=(kc > 0))
            T[ti] = dict(hk=hk, hq=hq)

        def stB(ti):
            d = T[ti]
            hk, hq = d["hk"], d["hq"]
            pks = []
            for nb in range(2):
                pk = g.psum.next()
                for kc in range(8):
                    k.mm(pk, pk[:, :], hk, hk[:, kc, :], kvw, kvw[:, kc, nb * 512:(nb + 1) * 512], start=(kc == 0), stop=(kc == 7))
                pks.append(pk)
            kn = kn_r.next()
            head_rmsnorm(k, pks, kn, None, kgb, (raw, sq, hs))
            pqs = []
            for nb in range(2):
                pq = g.psum.next()
                for kc in range(8):
                    k.mm(pq, pq[:, :], hq, hq[:, kc, :], wqg, wqg[:, kc, nb * 512:(nb + 1) * 512], start=(kc == 0), stop=(kc == 7))
                pqs.append(pq)
            qn = kn_r.next()
            head_rmsnorm(k, pqs, qn, None, qgb, (raw2, sq2, hs2))
            vb = vb_r.next()
            for nb in range(2):
                pv = g.psum.next()
                for kc in range(8):
                    k.mm(pv, pv[:, :], hk, hk[:, kc, :], kvw, kvw[:, kc, 1024 + nb * 512:1024 + (nb + 1) * 512],
                         start=(kc == 0), stop=(kc == 7))
                k.op("act", lambda e: e.copy(out=vb[:, nb * 512:(nb + 1) * 512], in_=pv[:, :]), reads=[pv], writes=[vb], join=(nb > 0))
            k.dma("act", V.ap[ti * 128:(ti + 1) * 128, :], vb[:, :], reads=[vb], writes=[])
            pf = g.psum.next()
            for kc in range(8):
                k.mm(pf, pf[:, 0:16], hk, hk[:, kc, :], kvw, kvw[:, kc, 2048:2064], start=(kc == 0), stop=(kc == 7))
            fz = fz_r.next()
            k.op("dve", lambda e: e.tensor_tensor(out=fz[:, 16:32], in0=pf[:, 0:16], in1=fgb[:, :], op=ALU.add),
                 reads=[pf, fgb], writes=[fz])
            k.op("act", lambda e: e.activation(out=fz[:, 16:32], in_=fz[:, 16:32], func=AF.Exp, scale=-1.0), reads=[fz], writes=[fz])
            k.op("act", lambda e: e.activation(out=fz[:, 32:48], in_=fz[:, 16:32], func=AF.Ln, bias=1.0, scale=1.0),
                 reads=[fz], writes=[fz])
            gT = gT_r.next()
            for c4 in range(2):
                pg = g.psum.next()
                for q in range(4):
                    c = c4 * 4 + q
                    for kc in range(8):
                        k.mm(pg, pg[:, q * 128:(q + 1) * 128], wqg, wqg[:, kc, 1024 + c * 128:1024 + (c + 1) * 128], hq, hq[:, kc, :],
                             start=(kc == 0), stop=(kc == 7))
                k.op("act", lambda e: e.activation(out=gT[:, c4 * 4:(c4 + 1) * 4, :], in_=pg[:, :].rearrange("p (q t) -> p q t", q=4),
                                                   func=AF.Sigmoid), reads=[pg], writes=[gT], join=(c4 > 0))
            k.dma("act", GT.ap[:, ti * 128:(ti + 1) * 128].rearrange("(c p) t -> p c t", p=128), gT[:, :, :], reads=[gT], writes=[])
            pc = g.psum.next()
            k.mm(pc, pc[:, 0:16], cf, cf[:, C_U:C_U + 128], fz, fz[:, 32:48], start=True, stop=True)
            k.mm(pc, pc[:, 16:32], cf, cf[:, C_ONE:C_ONE + 128], fz, fz[:, 32:48], start=True, stop=True)
            k.op("dve", lambda e: e.tensor_tensor(out=FP[:, ti, :], in0=pc[:, 0:16], in1=carry[:, :], op=ALU.add),
                 reads=[pc, carry], writes=[FP])
            k.op("dve", lambda e: e.tensor_tensor(out=carry[:, :], in0=pc[:, 16:32], in1=carry[:, :], op=ALU.add),
                 reads=[pc, carry], writes=[carry])
            pft = g.psum.next()
            k.tr(pft, pft[0:16, 0:128], FP, FP[:, ti, :], cf, cf[:, C_ID:C_ID + 128])
            fpt = fpt_r.next()
            k.op("dve", lambda e: e.tensor_copy(out=fpt[0:16, :], in_=pft[0:16, 0:128]), reads=[pft], writes=[fpt])
            k.dma("sp", FPT.ap[:, ti * 128:(ti + 1) * 128], fpt[0:16, :], reads=[fpt], writes=[])
            for src, dst in ((kn, KT), (qn, QT)):
                pt2 = psb_r.next()
                for c in range(8):
                    k.tr(pt2, pt2[:, c * 128:(c + 1) * 128], src, src[:, c * 128:(c + 1) * 128], cb, cb[:, B_ID:B_ID + 128])
                tT = tT_r.next()
                k.op("act", lambda e: e.copy(out=tT[:, :, :], in_=pt2[:, :].rearrange("p (c t) -> p c t", c=8)), reads=[pt2], writes=[tT])
                k.dma("sp", dst.ap[:, ti * 128:(ti + 1) * 128].rearrange("(c p) t -> p c t", p=128), tT[:, :, :], reads=[tT], writes=[])
            del T[ti]

        stL(0)
        stL(1)
        stA(0)
        for ti in range(NT):
            if ti + 2 < NT:
                stL(ti + 2)
            if ti + 1 < NT:
                stA(ti + 1)
            stB(ti)
        k.barrier()


def attn_phase(k, io, g, KT, QT, GT, V, FP, FPT, OT, heads=range(NH)):
    cf, cb = g.cf, g.cb
    QW = 512
    NQ = S // QW
    banks = g.psum.tiles
    sbanks = [banks[0], banks[1], banks[2]]
    extra = []
    for pb in (g.psb, g.psb2):
        t = Tk(pb.ap.bitcast(F32), pb.name + "_f32")
        extra.append(t)
    ps_s = Rot(sbanks + extra)
    ps_o = Rot(banks[3:5])
    ps_b = banks[5]
    LOOK = 4
    with contextlib.ExitStack() as st:
        kT_r = Rot([k.sb([65, S], BF16, stack=st, name="kTh") for _ in range(3)])
        qT_r = Rot([k.sb([65, S], BF16, stack=st, name="qTh") for _ in range(3)])
        fr = k.sb([65, S], F32, stack=st, name="fr")
        fr2 = k.sb([65, S], F32, stack=st, name="fr2")
        for kt_ in kT_r.tiles:
            k.op("pool", lambda e: e.memset(kt_[64:65, :], 1.0), writes=[kt_])
        gT_r = Rot([k.sb([64, S], BF16, stack=st, name="gTh") for _ in range(3)])
        Vh_r = Rot([k.sb([128, NT, 65], BF16, stack=st, name="Vh") for _ in range(3)])
        oT_r = Rot([k.sb([64, S], BF16, stack=st, name="oTh") for _ in range(3)])
        for vt in Vh_r.tiles:
            k.op("pool", lambda e: e.memset(vt[:, :, 64:65], 1.0), writes=[vt])
        Bq_r = Rot([k.sb([128, NQ, NT], F32, stack=st, name="Bq") for _ in range(3)])
        frb = k.sb([128, NQ], F32, stack=st, name="frb")
        pT_r = Rot([k.sb([128, QW], BF16, stack=st, name="pT") for _ in range(LOOK + 2)])
        rl_r = Rot([k.sb([128, QW], F32, stack=st, name="rl") for _ in range(2)])
        o1_r = Rot([k.sb([64, QW], F32, stack=st, name="o1") for _ in range(2)])
        HL = {}

        def hload(h):
            kT = kT_r.next()
            qT = qT_r.next()
            gT = gT_r.next()
            Vh = Vh_r.next()
            oT = oT_r.next()
            k.dma("sp", kT[0:64, :], KT.ap[h * 64:(h + 1) * 64, :], writes=[kT])
            k.dma("sp", qT[0:64, :], QT.ap[h * 64:(h + 1) * 64, :], writes=[qT])
            k.dma("sp", fr[64:65, :], FPT.ap[h:h + 1, :], writes=[fr])
            k.op("dve", lambda e: e.tensor_tensor(out=fr2[64:65, :].rearrange("p (q w) -> p q w", w=QW),
                                                  in0=fr[64:65, :].rearrange("p (q w) -> p q w", w=QW)[:, :, QW - 1:QW].to_broadcast([1, NQ, QW]),
                                                  in1=fr[64:65, :].rearrange("p (q w) -> p q w", w=QW), op=ALU.subtract),
                 reads=[fr], writes=[fr2])
            k.op("dve", lambda e: e.tensor_scalar(out=qT[64:65, :], in0=fr2[64:65, :], scalar1=8.0, scalar2=None, op0=ALU.mult),
                 reads=[fr2], writes=[qT], join=True)
            k.dma("sp", gT[:, :], GT.ap[h * 64:(h + 1) * 64, :], writes=[gT])
            k.dma("sp", Vh[:, :, 0:64], V.ap[:, h * 64:(h + 1) * 64].rearrange("(kb p) d -> p kb d", p=128), writes=[Vh])
            k.mm(ps_b, ps_b[:, 0:NQ], cf, cf[:, C_S127:C_S127 + 128], FP, FP[:, :, h].rearrange("p (a b) -> p a b", b=4)[:, :, 3],
                 start=True, stop=True)
            k.op("dve", lambda e: e.tensor_copy(out=frb[:, :], in_=ps_b[:, 0:NQ]), reads=[ps_b], writes=[frb])
            Bq = Bq_r.next()
            k.op("dve", lambda e: e.tensor_tensor(out=Bq[:, :, :], in0=FP[:, :, h].unsqueeze(1).to_broadcast([128, NQ, NT]),
                                                  in1=frb[:, :].unsqueeze(2).to_broadcast([128, NQ, NT]), op=ALU.subtract),
                 reads=[FP, frb], writes=[Bq])
            HL[h] = (kT, qT, gT, Vh, oT, Bq)

        heads = list(heads)
        hload(heads[0])
        for hi, h in enumerate(heads):
            kT, qT, gT, Vh, oT, Bq = HL.pop(h)
            if hi + 1 < len(heads):
                hload(heads[hi + 1])
            pairs = [(qb, kb) for qb in range(NQ) for kb in range(4 * qb + 4)]
            state = {}
            deferred = []

            def emit_qk(i):
                qb, kb = pairs[i]
                nkb = 4 * qb + 4
                pS = ps_s.next()
                diag = kb >= nkb - 4
                k.mm(pS, pS[:, 0:QW], kT, kT[0:65, kb * 128:(kb + 1) * 128], qT, qT[0:65, qb * QW:(qb + 1) * QW],
                     start=True, stop=not diag)
                if diag:
                    m0 = B_NM + 512 * (kb - (nkb - 4))
                    k.mm(pS, pS[:, 0:QW], cb, cb[:, B_NI:B_NI + 128], cb, cb[:, m0:m0 + QW], start=False, stop=True)
                pT = pT_r.next()
                k.op("act", lambda e: e.activation(out=pT[:, :], in_=pS[:, 0:QW], func=AF.Exp, scale=0.125,
                                                   bias=Bq[:, qb, kb:kb + 1]), reads=[pS, Bq], writes=[pT])
                state[i] = pT

            def emit_pv(i):
                qb, kb = pairs[i]
                nkb = 4 * qb + 4
                if kb == 0:
                    state["po"] = ps_o.next()
                po = state["po"]
                pT = state.pop(i)
                k.mm(po, po[0:65, 0:QW], Vh, Vh[:, kb, 0:65], pT, pT[:, :], start=(kb == 0), stop=(kb == nkb - 1))
                if kb == nkb - 1:
                    rl = rl_r.next()
                    o1 = o1_r.next()
                    k.op("dve", lambda e: e.reciprocal(out=rl[64:65, :], in_=po[64:65, 0:QW]), reads=[po], writes=[rl])
                    k.op("dve", lambda e: e.tensor_copy(out=o1[:, :], in_=po[0:64, 0:QW]), reads=[po], writes=[o1])

                    def fin(rl=rl, o1=o1, qb=qb):
                        k.mm(ps_b, ps_b[0:64, 0:QW], cf, cf[64:65, C_ONE:C_ONE + 64], rl, rl[64:65, :], start=True, stop=True)
                        k.op("dve", lambda e: e.tensor_tensor(out=o1[:, :], in0=o1[:, :], in1=ps_b[0:64, 0:QW], op=ALU.mult),
                             reads=[o1, ps_b], writes=[o1])
                        k.op("pool", lambda e: e.tensor_tensor(out=oT[:, qb * QW:(qb + 1) * QW], in0=o1[:, :],
                                                               in1=gT[:, qb * QW:(qb + 1) * QW], op=ALU.mult),
                             reads=[o1, gT], writes=[oT], join=(qb > 0))
                    deferred.append([3, fin])

            for i in range(len(pairs) + LOOK):
                if i < len(pairs):
                    emit_qk(i)
                if i >= LOOK:
                    emit_pv(i - LOOK)
                for dfr in list(deferred):
                    dfr[0] -= 1
                    if dfr[0] <= 0:
                        dfr[1]()
                        deferred.remove(dfr)
            for dfr in deferred:
                dfr[1]()
            deferred.clear()
            k.dma("sp", OT.ap[h * 64:(h + 1) * 64, :], oT[:, :], reads=[oT], writes=[])
        k.barrier()


def wo_phase(k, io, g, X_in, OT, X_out):
    gate = g.gates[1][0]
    with contextlib.ExitStack() as st:
        wo = k.sb([128, 8, 1024], BF16, stack=st, name="wo")
        for kc in range(8):
            k.dma("pool", wo[:, kc, :], io["wo"][kc * 128:(kc + 1) * 128, :], writes=[wo])
        xt_r = Rot([k.sb([128, 1024], F32, stack=st, name="wxt") for _ in range(4)])
        o_r = Rot([k.sb([128, 8, 128], BF16, stack=st, name="wo_o") for _ in range(4)])
        y_r = Rot([k.sb([128, 1024], F32, stack=st, name="wy") for _ in range(2)])
        WL = {}

        def wl(ti):
            xt = xt_r.next()
            k.dma("sp", xt[:, :], X_in.ap[ti * 128:(ti + 1) * 128, :], reads=[X_in], writes=[xt])
            o = o_r.next()
            k.dma("act", o[:, :, :], OT.ap[:, ti * 128:(ti + 1) * 128].rearrange("(c p) t -> p c t", p=128), writes=[o])
            WL[ti] = (xt, o)

        wl(0)
        wl(1)
        for ti in range(NT):
            if ti + 2 < NT:
                wl(ti + 2)
            xt, o = WL.pop(ti)
            y = y_r.next()
            for nb in range(2):
                py = g.psum.next()
                for c in range(8):
                    k.mm(py, py[:, :], o, o[:, c, :], wo, wo[:, c, nb * 512:(nb + 1) * 512], start=(c == 0), stop=(c == 7))
                k.op("dve", lambda e: e.tensor_tensor(out=y[:, nb * 512:(nb + 1) * 512], in0=py[:, :],
                                                      in1=gate[:, nb * 512:(nb + 1) * 512], op=ALU.mult),
                     reads=[py, gate], writes=[y], join=(nb > 0))
            k.op("pool", lambda e: e.tensor_tensor(out=y[:, :], in0=y[:, :], in1=xt[:, :], op=ALU.add), reads=[y, xt], writes=[y])
            k.dma("sp", X_out.ap[ti * 128:(ti + 1) * 128, :], y[:, :], reads=[y], writes=[])
        k.barrier()


def prep_inputs(inp, b):
    cf, cb = make_consts()
    f = lambda a: np.ascontiguousarray(a, dtype=np.float32)
    return dict(
        x=f(inp["x"][b]), c8=f(inp["c"][b].reshape(8, 128)), ada_w=f(inp["ada_w"].reshape(2048, 6144)), ada_b=f(inp["ada_b"]),
        nmg=f(inp["norm_mix_g"].reshape(16, 128)), nfg=f(inp["norm_ffn_g"].reshape(16, 128)),
        gw_in=f(inp["gmlp_w_in"][0]), gvg=f(inp["gmlp_v_g"].reshape(16, 128)), gvb=f(inp["gmlp_v_b"].reshape(1, 2048)),
        gws=f(inp["gmlp_w_s"][0]), gbs=f(inp["gmlp_b_s"].reshape(1, 1024)), gw_out=f(inp["gmlp_w_out"][0]),
        kvaw=f(inp["kv_ada_w"]), kvab=f(inp["kv_ada_b"].reshape(1, 2048)), kvng=f(inp["kv_norm_g"].reshape(8, 128)),
        kvw=f(inp["kv_w"]), kng=f(inp["k_norm_g"].reshape(1, 64)), fgb=f(inp["fgate_b"].reshape(1, 16)),
        wqg=f(inp["fox_w_qg"][0]), qng=f(inp["q_norm_g"].reshape(1, 64)), wo=f(inp["fox_w_o"][0]),
        rw=f(inp["router_w"].reshape(2048, 32)), rb=f(inp["router_b"]),
        w1=f(inp["exp_w1"].reshape(65536, 2048)), b1=f(inp["exp_b1"].reshape(64, 16, 128).transpose(0, 2, 1).reshape(64 * 128, 16)),
        w2=f(inp["exp_w2"].reshape(65536, 1024)), b2=f(inp["exp_b2"].reshape(64, 1024)),
        fing=f(inp["final_g"].reshape(1, 1024)), cf=cf, cb=cb,
    )


_NC_CACHE = {}


def build_program():
    nc = bass.Bass("TRN2", target_bir_lowering=False)
    with contextlib.ExitStack() as stack:
        k = K(nc, stack)
        io = declare_inputs(nc)
        g = G()
        setup(k, io, g)
        R = RState(k)
        FP = k.sb([128, NT, 16], F32, name="FP")
        X0 = Tk(io["x"], "x")
        X1 = k.dram("X1", [S, D], F32)
        X2 = k.dram("X2", [S, D], F32)
        X3 = k.dram("X3", [S, D], F32)
        OUT = k.dram("out", [S, D], F32, kind="ExternalOutput")
        BUF = k.dram("BUF", [32 * CAP, D], BF16)
        Y = k.dram("Y", [NB * 256, D], F32)
        KT = k.dram("KT", [D, S], BF16)
        QT = k.dram("QT", [D, S], BF16)
        GT = k.dram("GT", [D, S], BF16)
        V = k.dram("V", [S, D], BF16)
        OT = k.dram("OT", [D, S], BF16)
        FPT = k.dram("FPT", [16, S], F32)
        W1B = Tk(io["w1"], "w1")
        W2B = Tk(io["w2"], "w2")
        gmlp_phase(k, io, g, X0, X1)
        route_phase(k, io, g, 0, X1, BUF, R)
        expert_phase(k, io, g, 0, BUF, Y, R, W1B, W2B)
        combine_phase(k, io, g, 0, X1, Y, X2, R)
        kvq_phase(k, io, g, X2, KT, QT, GT, V, FP, FPT)
        attn_phase(k, io, g, KT, QT, GT, V, FP, FPT, OT)
        wo_phase(k, io, g, X2, OT, X3)
        route_phase(k, io, g, 1, X3, BUF, R)
        expert_phase(k, io, g, 1, BUF, Y, R, W1B, W2B)
        combine_phase(k, io, g, 1, X3, Y, OUT, R, final=True)
        k.barrier()
    return nc


def kernel(**inputs):
    inp = {kk: np.asarray(v) for kk, v in inputs.items()}
    if "nc" not in _NC_CACHE:
        _NC_CACHE["nc"] = build_program()
    nc = _NC_CACHE["nc"]
    shared = prep_inputs(inp, 0)
    in_maps = []
    for b in range(8):
        m = dict(shared)
        m["x"] = np.ascontiguousarray(inp["x"][b], dtype=np.float32)
        m["c8"] = np.ascontiguousarray(inp["c"][b].reshape(8, 128), dtype=np.float32)
        in_maps.append(m)
    res = run_bass_kernel_spmd(nc, in_maps, core_ids=list(range(8)))
    out = np.stack([np.asarray(r["out"], dtype=np.float32) for r in res.results], axis=0)
    return out
```

```python
import contextlib
import os
import numpy as np
import ml_dtypes
import concourse.bass as bass
import concourse.mybir as mybir
from concourse.bass_utils import run_bass_kernel_spmd


F32 = mybir.dt.float32
BF16 = mybir.dt.bfloat16
I32 = mybir.dt.int32
AF = mybir.ActivationFunctionType
ALU = mybir.AluOpType
AX = mybir.AxisListType


class Tk:
    __slots__ = ("ap", "w", "r", "g", "name")

    def __init__(self, ap, name=""):
        self.ap = ap
        self.w = {}
        self.r = {}
        self.g = {}
        self.name = name

    def __getitem__(self, key):
        return self.ap[key]


class K:
    NDS = {"sp": 30, "act": 22, "pool": 44}

    def __init__(self, nc, stack):
        self.nc = nc
        self.stack = stack
        self.eng = dict(pe=nc.tensor, act=nc.scalar, dve=nc.vector, pool=nc.gpsimd, sp=nc.sync)
        self.sems = {}
        for e in self.eng:
            self.sems[("c", e)] = stack.enter_context(nc.semaphore("c_" + e))
        for e in ("sp", "act", "pool"):
            for i in range(self.NDS[e]):
                self.sems[("d", e, i)] = stack.enter_context(nc.semaphore("d_%s%d" % (e, i)))
        self.cnt = {s: 0 for s in self.sems}
        self.dn = {e: 0 for e in ("sp", "act", "pool")}
        self.waited = {}
        self.uid = 0
        self.ninst = 0

    def sb(self, shape, dtype, stack=None, name=None):
        self.uid += 1
        nm = "%s_%d" % (name or "t", self.uid)
        t = (stack or self.stack).enter_context(self.nc.sbuf_tensor(nm, list(shape), dtype))
        return Tk(t, nm)

    def ps(self, shape, dtype, stack=None, name=None):
        self.uid += 1
        nm = "%s_%d" % (name or "p", self.uid)
        t = (stack or self.stack).enter_context(self.nc.psum_tensor(nm, list(shape), dtype))
        return Tk(t, nm)

    def dram(self, name, shape, dtype, kind="Internal"):
        t = self.nc.dram_tensor(name, list(shape), dtype, kind=kind).ap()
        return Tk(t, name)

    def op(self, eng, fn, reads=(), writes=(), dma=False, join=False):
        deps = {}

        def add(s, v):
            if deps.get(s, 0) < v:
                deps[s] = v

        for t in reads:
            for s, v in t.w.items():
                add(s, v)
        for t in writes:
            if join:
                for s, v in t.g.items():
                    add(s, v)
            else:
                t.g = dict(t.w)
                for s, v in t.r.items():
                    if t.g.get(s, 0) < v:
                        t.g[s] = v
                for s, v in t.w.items():
                    add(s, v)
            for s, v in t.r.items():
                add(s, v)
        e = self.eng[eng]
        for s, v in deps.items():
            if eng == "pe" and s == ("c", "pe"):
                continue
            if self.waited.get((eng, s), 0) >= v:
                continue
            e.wait_ge(self.sems[s], v)
            self.waited[(eng, s)] = v
            self.ninst += 1
        if dma:
            i = self.dn[eng]
            self.dn[eng] += 1
            skey = ("d", eng, i % self.NDS[eng])
            inc = 16
            prev = self.cnt[skey]
            if prev > 0 and self.waited.get((eng, skey), 0) < prev:
                e.wait_ge(self.sems[skey], prev)
                self.waited[(eng, skey)] = prev
                self.ninst += 1
        ins = fn(e)
        self.ninst += 1
        if dma:
            pass
        else:
            skey = ("c", eng)
            inc = 1
        ins.then_inc(self.sems[skey], inc)
        self.cnt[skey] += inc
        val = self.cnt[skey]
        for t in reads:
            if t.r.get(skey, 0) < val:
                t.r[skey] = val
        for t in writes:
            if join:
                if t.w.get(skey, 0) < val:
                    t.w[skey] = val
            else:
                t.w = {skey: val}
                t.r = {}
        return (skey, val)

    def barrier(self, engines=None):
        for eng in (engines or self.eng):
            e = self.eng[eng]
            for s, v in self.cnt.items():
                if v == 0 or self.waited.get((eng, s), 0) >= v:
                    continue
                e.wait_ge(self.sems[s], v)
                self.waited[(eng, s)] = v
                self.ninst += 1

    def dma(self, eng, out_ap, in_ap, reads=(), writes=(), join=False, **kw):
        return self.op(eng, lambda e: e.dma_start(out=out_ap, in_=in_ap, **kw), reads, writes, dma=True, join=join)

    def mm(self, out_t, out_ap, l_t, l_ap, r_t, r_ap, start=True, stop=True):
        return self.op("pe", lambda e: e.matmul(out_ap, l_ap, r_ap, start=start, stop=stop),
                       reads=[l_t, r_t], writes=[out_t])

    def tr(self, out_t, out_ap, in_t, in_ap, id_t, id_ap):
        return self.op("pe", lambda e: e.transpose(out_ap, in_ap, id_ap), reads=[in_t, id_t], writes=[out_t])


class Rot:
    def __init__(self, tiles):
        self.tiles = tiles
        self.i = 0

    def next(self):
        t = self.tiles[self.i % len(self.tiles)]
        self.i += 1
        return t


S = 4096
D = 1024
NT = 32
EPS = 1e-6
NCF = 900
NCB = 640 + 2048
C_ID, C_U, C_US, C_L, C_ONE, C_S127, C_IE, C_IP, C_IB = 0, 128, 256, 384, 512, 640, 768, 800, 801
B_ID, B_MA, B_ONE, B_NI, B_NM = 0, 128, 384, 512, 640


def make_consts():
    cf = np.zeros((128, NCF), np.float32)
    p = np.arange(128)[:, None]
    f = np.arange(128)[None, :]
    cf[:, C_ID:C_ID + 128] = (p == f)
    cf[:, C_U:C_U + 128] = (p <= f)
    cf[:, C_US:C_US + 128] = (p < f)
    cf[:, C_L:C_L + 128] = (f <= p)
    cf[:, C_ONE:C_ONE + 128] = 1.0
    cf[127, C_S127:C_S127 + 128] = 1.0
    cf[:, C_IE:C_IE + 32] = np.arange(32)[None, :]
    cf[:, C_IP] = np.arange(128)
    cf[:, C_IB:C_IB + 96] = np.arange(96)[None, :]
    cb = np.zeros((128, NCB), np.float32)
    cb[:, B_ID:B_ID + 128] = (p == f)
    f2 = np.arange(256)[None, :]
    cb[:, B_MA:B_MA + 256] = (p <= f2)
    cb[:, B_ONE:B_ONE + 128] = 1.0
    cb[:, B_NI:B_NI + 128] = -30000.0 * (p == f)
    f5 = np.arange(512)[None, :]
    for j in range(4):
        cb[:, B_NM + 512 * j:B_NM + 512 * (j + 1)] = (p + 128 * j > f5)
    return cf, cb.astype(ml_dtypes.bfloat16)


def declare_inputs(nc, only=None):
    def I(name, shape, dt=F32):
        if only is not None and name not in only:
            return None
        return nc.dram_tensor(name, list(shape), dt, kind="ExternalInput").ap()
    io = dict(
        x=I("x", [S, D]), c8=I("c8", [8, 128]), ada_w=I("ada_w", [2048, 6144]), ada_b=I("ada_b", [2, 6144]),
        nmg=I("nmg", [16, 128]), nfg=I("nfg", [16, 128]), gw_in=I("gw_in", [1024, 4096]), gvg=I("gvg", [16, 128]),
        gvb=I("gvb", [1, 2048]), gws=I("gws", [8, 128, 128]), gbs=I("gbs", [1, 1024]), gw_out=I("gw_out", [2048, 1024]),
        kvaw=I("kvaw", [1024, 2048]), kvab=I("kvab", [1, 2048]), kvng=I("kvng", [8, 128]), kvw=I("kvw", [1024, 2064]),
        kng=I("kng", [1, 64]), fgb=I("fgb", [1, 16]), wqg=I("wqg", [1024, 2048]), qng=I("qng", [1, 64]),
        wo=I("wo", [1024, 1024]), rw=I("rw", [2048, 32]), rb=I("rb", [2, 32]),
        w1=I("w1", [65536, 2048]), b1=I("b1", [64 * 128, 16]), w2=I("w2", [65536, 1024]), b2=I("b2", [64, 1024]),
        fing=I("fing", [1, 1024]), cf=I("cf", [128, NCF]), cb=I("cb", [128, NCB], BF16),
    )
    return io


class G:
    pass


def setup(k, io, g):
    nc = k.nc
    g.cf = k.sb([128, NCF], F32, name="cf")
    g.cb = k.sb([128, NCB], BF16, name="cb")
    k.dma("sp", g.cf[:, :], io["cf"][:, :], writes=[g.cf])
    k.dma("sp", g.cb[:, :], io["cb"][:, :], writes=[g.cb])
    g.psum = Rot([k.ps([128, 512], F32, name="pb%d" % i) for i in range(6)])
    g.psb = k.ps([128, 1024], BF16, name="pbb")
    g.psb2 = k.ps([128, 1024], BF16, name="pbb2")
    cf = g.cf
    g.bc_w = nc.gpsimd.to_reg(65535)
    g.bc_b = nc.gpsimd.to_reg(63)
    g.bc_c = nc.gpsimd.to_reg(64 * 128 - 1)
    rows = k.sb([64, 128], F32, name="rows")
    srcs = [(io["nmg"][0:8, :], 0), (io["nfg"][0:8, :], 8), (io["kvng"][:, :], 16), (io["nmg"][8:16, :], 24),
            (io["nfg"][8:16, :], 32), (io["c8"][:, :], 40), (io["gvg"][:, :], 48)]
    for ap, r0 in srcs:
        n = ap.shape[0]
        k.dma("sp", rows[r0:r0 + n, :], ap, writes=[rows])
    pt = g.psum.next()
    k.tr(pt, pt[:, 0:64], rows, rows[0:64, :], cf, cf[0:64, C_ID:C_ID + 64])
    g.cols = k.sb([128, 64], F32, name="cols")
    k.op("dve", lambda e: e.tensor_copy(out=g.cols[:, :], in_=pt[:, 0:64]), reads=[pt], writes=[g.cols])
    silu = k.sb([128, 8], F32, name="silu")
    k.op("act", lambda e: e.activation(out=silu[:, :], in_=g.cols[:, 40:48], func=AF.Silu), reads=[g.cols], writes=[silu])
    one11 = cf[0:1, C_ONE:C_ONE + 1]
    ones_row = cf[0:1, C_ONE:C_ONE + 128]
    g.modT = [k.sb([128, 48], F32, name="modT%d" % l) for l in range(2)]
    g.modK = k.sb([128, 16], F32, name="modK")
    g.gates = [[k.sb([128, 1024], F32, name="gate%d%d" % (l, w)) for w in range(2)] for l in range(2)]
    with contextlib.ExitStack() as st:
        silu_rep = k.sb([128, 8, 128], F32, stack=st, name="silurep")
        k.op("dve", lambda e: e.tensor_copy(out=silu_rep[:, :, :], in_=silu[:, :].unsqueeze(2).to_broadcast([128, 8, 128])),
             reads=[silu], writes=[silu_rep])
        wch = Rot([k.sb([128, 8, 512], F32, stack=st, name="wch") for _ in range(2)])
        bch = Rot([k.sb([1, 512], F32, stack=st, name="bch") for _ in range(2)])
        groups = [(io["ada_w"][0:1024, :], io["ada_b"][0:1, :], 12, g.modT[0], 0),
                  (io["ada_w"][1024:2048, :], io["ada_b"][1:2, :], 12, g.modT[1], 1),
                  (io["kvaw"][:, :], io["kvab"][0:1, :], 4, g.modK, None)]
        for W, Bv, nch, dst, l in groups:
            pm = g.psum.next()
            for cc in range(nch):
                w = wch.next()
                b = bch.next()
                k.dma("sp" if cc % 2 == 0 else "act", w[:, :, :],
                      W[:, cc * 512:(cc + 1) * 512].rearrange("(k p) n -> p k n", p=128), writes=[w])
                k.dma("sp", b[:, :], Bv[:, cc * 512:(cc + 1) * 512], writes=[b])
                for q in range(4):
                    j = cc * 4 + q
                    for kc in range(8):
                        k.mm(pm, pm[:, j:j + 1], w, w[:, kc, q * 128:(q + 1) * 128], silu, silu[:, kc:kc + 1],
                             start=(kc == 0), stop=False)
                    k.mm(pm, pm[:, j:j + 1], b, b[0:1, q * 128:(q + 1) * 128], cf, one11, start=False, stop=True)
                if l is not None and cc in (4, 5, 10, 11):
                    pg = g.psum.next()
                    for kc in range(8):
                        k.mm(pg, pg[:, :], silu_rep, silu_rep[:, kc, :], w, w[:, kc, :], start=(kc == 0), stop=False)
                    k.mm(pg, pg[:, :], cf, ones_row, b, b[0:1, :], start=False, stop=True)
                    gt = g.gates[l][0 if cc < 6 else 1]
                    half = cc % 2
                    k.op("act", lambda e: e.copy(out=gt[:, half * 512:(half + 1) * 512], in_=pg[:, :]),
                         reads=[pg], writes=[gt])
            k.op("dve", lambda e: e.tensor_copy(out=dst[:, 0:nch * 4], in_=pm[:, 0:nch * 4]), reads=[pm], writes=[dst])
        k.barrier()
    g.A = k.sb([128, 5, 8], F32, name="A")
    specs = [(0, g.modT[0], 8, 0), (1, g.modT[0], 32, 8), (2, g.modK, 8, 16), (3, g.modT[1], 8, 24), (4, g.modT[1], 32, 32)]
    g.Bsh = []
    for n, mt, sc0, g0 in specs:
        k.op("dve", lambda e: e.scalar_tensor_tensor(out=g.A[:, n, :], in0=mt[:, sc0:sc0 + 8], scalar=1.0,
                                                     in1=g.cols[:, g0:g0 + 8], op0=ALU.add, op1=ALU.mult),
             reads=[mt, g.cols], writes=[g.A])
        g.Bsh.append((mt, sc0 - 8))
    return g


def rms_rstd(k, x_t, x_ap, junk, ss, rstd):
    k.op("act", lambda e: e.activation(out=junk[:, :], in_=x_ap, func=AF.Square, accum_out=ss[:, 0:1]),
         reads=[x_t], writes=[junk, ss])
    k.op("dve", lambda e: e.tensor_scalar(out=rstd[:, 0:1], in0=ss[:, 0:1], scalar1=1.0 / D, scalar2=EPS,
                                          op0=ALU.mult, op1=ALU.add), reads=[ss], writes=[rstd])
    k.op("act", lambda e: e.activation(out=rstd[:, 0:1], in_=rstd[:, 0:1], func=AF.Sqrt), reads=[rstd], writes=[rstd])
    k.op("dve", lambda e: e.reciprocal(out=rstd[:, 0:1], in_=rstd[:, 0:1]), reads=[rstd], writes=[rstd])


def gmlp_phase(k, io, g, X_in, X_out, dbg=None, after_loads=None):
    cf, cb = g.cf, g.cb
    psum7 = Rot(list(g.psum.tiles) + [Tk(g.psb2.ap.bitcast(F32), 'psb2_f32')])
    with contextlib.ExitStack() as st:
        w_in = k.sb([128, 8, 4096], BF16, stack=st, name="w_in")
        w_out = k.sb([128, 16, 1024], BF16, stack=st, name="w_out")
        for kc in range(8):
            k.dma("pool", w_in[:, kc, :], io["gw_in"][kc * 128:(kc + 1) * 128, :], writes=[w_in])
        for kc in range(16):
            k.dma("pool", w_out[:, kc, :], io["gw_out"][kc * 128:(kc + 1) * 128, :], writes=[w_out])
        if after_loads is not None:
            after_loads()
        WcT = k.sb([128, 8, 128], BF16, stack=st, name="WcT")
        Cc = k.sb([128, 16, 128], F32, stack=st, name="Cc")
        st2 = contextlib.ExitStack()
        ws_nat = k.sb([128, 8, 128], F32, stack=st2, name="ws_nat")
        k.dma("sp", ws_nat[:, :, :], io["gws"].rearrange("g t s -> t g s"), writes=[ws_nat])
        ws_m = k.sb([128, 8, 128], BF16, stack=st2, name="ws_m")
        k.op("dve", lambda e: e.tensor_tensor(out=ws_m[:, :, :], in0=ws_nat[:, :, :],
                                              in1=cf[:, C_L:C_L + 128].unsqueeze(1).to_broadcast([128, 8, 128]),
                                              op=ALU.mult), reads=[ws_nat, cf], writes=[ws_m])
        for gi in range(8):
            k.tr(g.psb, g.psb[:, gi * 128:(gi + 1) * 128], ws_m, ws_m[:, gi, :], cb, cb[:, B_ID:B_ID + 128])
        k.op("dve", lambda e: e.tensor_copy(out=WcT[:, :, :], in_=g.psb[:, :].rearrange("p (g t) -> p g t", g=8)),
             reads=[g.psb], writes=[WcT])
        rs_row = k.sb([1, 1024], F32, stack=st2, name="rs_row")
        for hf in range(2):
            pr = psum7.next()
            k.mm(pr, pr[0:1, :], cb, cb[:, B_ONE:B_ONE + 1], WcT, WcT[:, hf * 4:(hf + 1) * 4, :], start=True, stop=True)
            k.op("dve", lambda e: e.tensor_copy(out=rs_row[0:1, hf * 512:(hf + 1) * 512], in_=pr[0:1, :]),
                 reads=[pr], writes=[rs_row])
        bs_row = k.sb([1, 1024], F32, stack=st2, name="bs_row")
        vb_row = k.sb([1, 2048], F32, stack=st2, name="vb_row")
        k.dma("sp", bs_row[:, :], io["gbs"][:, :], writes=[bs_row])
        k.dma("sp", vb_row[:, :], io["gvb"][:, :], writes=[vb_row])
        ones_row = cf[0:1, C_ONE:C_ONE + 128]
        for fo4 in range(4):
            pc = psum7.next()
            for q in range(4):
                fo = fo4 * 4 + q
                gi = fo // 2
                k.mm(pc, pc[:, q * 128:(q + 1) * 128], vb_row, vb_row[0:1, fo * 128:(fo + 1) * 128],
                     rs_row, rs_row[0:1, gi * 128:(gi + 1) * 128], start=True, stop=False)
                k.mm(pc, pc[:, q * 128:(q + 1) * 128], cf, ones_row, bs_row, bs_row[0:1, gi * 128:(gi + 1) * 128],
                     start=False, stop=True)
            k.op("dve", lambda e: e.tensor_copy(out=Cc[:, fo4 * 4:(fo4 + 1) * 4, :],
                                                in_=pc[:, :].rearrange("p (q t) -> p q t", q=4)),
                 reads=[pc], writes=[Cc])
        k.barrier()
        st2.close()
        gam = g.cols
        xt_r = Rot([k.sb([128, 1024], F32, stack=st, name="xt") for _ in range(4)])
        ss_r = Rot([k.sb([128, 2], F32, stack=st, name="ss") for _ in range(2)])
        xh_r = Rot([k.sb([128, 1024], BF16, stack=st, name="xh") for _ in range(2)])
        hT_r = Rot([k.sb([128, 8, 128], BF16, stack=st, name="hT") for _ in range(2)])
        v_r = Rot([k.sb([128, 2048], F32, stack=st, name="v") for _ in range(1)])
        st_r = Rot([k.sb([128, 32], F32, stack=st, name="st") for _ in range(2)])
        vh_r = Rot([k.sb([128, 2048], BF16, stack=st, name="vh") for _ in range(2)])
        uT_r = Rot([k.sb([128, 16, 128], F32, stack=st, name="uT") for _ in range(1)])
        tmp_r = Rot([k.sb([128, 4, 128], F32, stack=st, name="tmp") for _ in range(1)])
        gT_r = Rot([k.sb([128, 16, 128], BF16, stack=st, name="gT") for _ in range(2)])
        x1_r = Rot([k.sb([128, 1024], F32, stack=st, name="x1") for _ in range(2)])
        A0 = g.A
        Bt, Bc = g.Bsh[0]
        gate = g.gates[0][0]
        T = {}

        def stL(ti):
            d = T.setdefault(ti, {})
            xt = xt_r.next()
            k.dma("sp", xt[:, :], X_in.ap[ti * 128:(ti + 1) * 128, :], reads=[X_in], writes=[xt])
            d["xt"] = xt

        def stA(ti):
            d = T[ti]
            xt = d["xt"]
            ss = ss_r.next()
            xh = xh_r.next()
            rms_rstd(k, xt, xt[:, :], xh, ss, ss)
            k.op("act", lambda e: e.activation(out=xh[:, :], in_=xt[:, :], func=AF.Copy, scale=ss[:, 0:1]),
                 reads=[xt, ss], writes=[xh])
            for kc in range(8):
                k.tr(g.psb, g.psb[:, kc * 128:(kc + 1) * 128], xh, xh[:, kc * 128:(kc + 1) * 128], cb, cb[:, B_ID:B_ID + 128])
            hT = hT_r.next()
            for kc in range(8):
                eng = "dve" if kc % 2 == 0 else "pool"
                if eng == "pool":
                    eng = "dve"
                k.op(eng, lambda e: e.tensor_scalar(out=hT[:, kc, :], in0=g.psb[:, kc * 128:(kc + 1) * 128],
                                                    scalar1=A0[:, 0, kc:kc + 1], scalar2=Bt[:, Bc + kc:Bc + kc + 1],
                                                    op0=ALU.mult, op1=ALU.add), reads=[g.psb, A0, Bt], writes=[hT], join=(kc > 0))
            d.update(xt=xt, hT=hT)

        def stB(ti):
            d = T[ti]; hT = d['hT']
            v = v_r.next()
            for nb in range(4):
                pv = psum7.next()
                for kc in range(8):
                    k.mm(pv, pv[:, :], hT, hT[:, kc, :], w_in, w_in[:, kc, 2048 + nb * 512:2048 + (nb + 1) * 512],
                         start=(kc == 0), stop=(kc == 7))
                k.op("act", lambda e: e.activation(out=v[:, nb * 512:(nb + 1) * 512], in_=pv[:, :], func=AF.Gelu),
                     reads=[pv], writes=[v], join=(nb > 0))
            stt = st_r.next()
            for nb in range(4):
                k.op("dve", lambda e: e.bn_stats(out=stt[:, nb * 6:(nb + 1) * 6], in_=v[:, nb * 512:(nb + 1) * 512]),
                     reads=[v], writes=[stt])
            k.op("dve", lambda e: e.bn_aggr(out=stt[:, 24:26], in_=stt[:, 0:24]), reads=[stt], writes=[stt])
            k.op("dve", lambda e: e.tensor_scalar(out=stt[:, 26:27], in0=stt[:, 25:26], scalar1=EPS, scalar2=None,
                                                  op0=ALU.add), reads=[stt], writes=[stt])
            k.op("act", lambda e: e.activation(out=stt[:, 26:27], in_=stt[:, 26:27], func=AF.Sqrt), reads=[stt], writes=[stt])
            k.op("dve", lambda e: e.reciprocal(out=stt[:, 26:27], in_=stt[:, 26:27]), reads=[stt], writes=[stt])
            vh = vh_r.next()
            k.op("dve", lambda e: e.tensor_scalar(out=vh[:, :], in0=v[:, :], scalar1=stt[:, 24:25], scalar2=stt[:, 26:27],
                                                  op0=ALU.subtract, op1=ALU.mult), reads=[v, stt], writes=[vh])
            uT = uT_r.next()
            for f4 in range(4):
                pu = psum7.next()
                for q in range(4):
                    fo = f4 * 4 + q
                    for kc in range(8):
                        k.mm(pu, pu[:, q * 128:(q + 1) * 128], w_in, w_in[:, kc, fo * 128:(fo + 1) * 128], hT, hT[:, kc, :],
                             start=(kc == 0), stop=(kc == 7))
                k.op("act", lambda e: e.activation(out=uT[:, f4 * 4:(f4 + 1) * 4, :],
                                                   in_=pu[:, :].rearrange("p (q t) -> p q t", q=4), func=AF.Gelu),
                     reads=[pu], writes=[uT], join=(f4 > 0))
            d.update(vh=vh, uT=uT)

        def stC(ti):
            d = T[ti]; vh = d['vh']; uT = d['uT']
            gT = gT_r.next()
            for f4 in range(4):
                pm = psum7.next()
                for q in range(4):
                    fo = f4 * 4 + q
                    k.mm(pm, pm[:, q * 128:(q + 1) * 128], vh, vh[:, fo * 128:(fo + 1) * 128], WcT, WcT[:, fo // 2, :],
                         start=True, stop=True)
                tmp = tmp_r.next()
                k.op("dve", lambda e: e.tensor_tensor(out=tmp[:, :, :], in0=pm[:, :].rearrange("p (q t) -> p q t", q=4),
                                                      in1=gam[:, 48 + f4 * 4:48 + (f4 + 1) * 4].unsqueeze(2).to_broadcast([128, 4, 128]),
                                                      op=ALU.mult), reads=[pm, gam], writes=[tmp])
                k.op("pool", lambda e: e.tensor_tensor(out=tmp[:, :, :], in0=tmp[:, :, :], in1=Cc[:, f4 * 4:(f4 + 1) * 4, :],
                                                       op=ALU.add), reads=[tmp, Cc], writes=[tmp])
                k.op("pool", lambda e: e.tensor_tensor(out=gT[:, f4 * 4:(f4 + 1) * 4, :], in0=tmp[:, :, :],
                                                       in1=uT[:, f4 * 4:(f4 + 1) * 4, :], op=ALU.mult),
                     reads=[tmp, uT], writes=[gT], join=(f4 > 0))
            d.update(gT=gT)

        def stD(ti):
            d = T[ti]; gT = d['gT']; xt = d['xt']
            x1 = x1_r.next()
            for nb in range(2):
                py = psum7.next()
                for fo in range(16):
                    k.mm(py, py[:, :], gT, gT[:, fo, :], w_out, w_out[:, fo, nb * 512:(nb + 1) * 512],
                         start=(fo == 0), stop=(fo == 15))
                k.op("dve", lambda e: e.tensor_tensor(out=x1[:, nb * 512:(nb + 1) * 512], in0=py[:, :],
                                                      in1=gate[:, nb * 512:(nb + 1) * 512], op=ALU.mult),
                     reads=[py, gate], writes=[x1], join=(nb > 0))
            k.op("pool", lambda e: e.tensor_tensor(out=x1[:, :], in0=x1[:, :], in1=xt[:, :], op=ALU.add),
                 reads=[x1, xt], writes=[x1])
            k.dma("sp", X_out.ap[ti * 128:(ti + 1) * 128, :], x1[:, :], reads=[x1], writes=[])

            del T[ti]

        stL(0)
        stL(1)
        stA(0)
        for ti in range(NT):
            if ti + 2 < NT:
                stL(ti + 2)
            if ti + 1 < NT:
                stA(ti + 1)
            stB(ti)
            stC(ti)
            if ti >= 1:
                stD(ti - 1)
        stD(NT - 1)
        k.barrier()


NB = 95
CAP = 4096


def IOA(ap):
    return bass.IndirectOffsetOnAxis(ap=ap, axis=0)


def route_phase(k, io, g, l, X_in, BUF, R):
    cf, cb = g.cf, g.cb
    n = 1 if l == 0 else 4
    Bt, Bc = g.Bsh[n]
    with contextlib.ExitStack() as st:
        Wr = k.sb([128, 8, 32], F32, stack=st, name="Wr")
        Wr2 = k.sb([128, 8, 32], F32, stack=st, name="Wr2")
        rbr = k.sb([1, 32], F32, stack=st, name="rbr")
        br2 = k.sb([1, 32], F32, stack=st, name="br2")
        carry = k.sb([128, 32], F32, stack=st, name="carry")
        k.dma("sp", Wr[:, :, :], io["rw"][l * 1024:(l + 1) * 1024, :].rearrange("(k p) e -> p k e", p=128), writes=[Wr])
        k.dma("sp", rbr[:, :], io["rb"][l:l + 1, :], writes=[rbr])
        k.op("dve", lambda e: e.tensor_tensor(out=Wr2[:, :, :], in0=Wr[:, :, :],
                                              in1=g.A[:, n, :].unsqueeze(2).to_broadcast([128, 8, 32]), op=ALU.mult),
             reads=[Wr, g.A], writes=[Wr2])
        pb = g.psum.next()
        for kc in range(8):
            k.mm(pb, pb[0:1, 0:32], Bt, Bt[:, Bc + kc:Bc + kc + 1], Wr, Wr[:, kc, :], start=(kc == 0), stop=False)
        k.mm(pb, pb[0:1, 0:32], cf, cf[0:1, C_ONE:C_ONE + 1], rbr, rbr[0:1, :], start=False, stop=True)
        k.op("dve", lambda e: e.tensor_copy(out=br2[:, :], in_=pb[0:1, 0:32]), reads=[pb], writes=[br2])
        k.op("dve", lambda e: e.memset(carry[:, :], 0.0), writes=[carry])
        xt_r = Rot([k.sb([128, 1024], F32, stack=st, name="rxt") for _ in range(4)])
        junk = k.sb([128, 1024], BF16, stack=st, name="rjunk")
        ss_r = Rot([k.sb([128, 2], F32, stack=st, name="rss") for _ in range(3)])
        xh_r = Rot([k.sb([128, 1024], F32, stack=st, name="rxh") for _ in range(3)])
        xb_r = Rot([k.sb([128, 1024], BF16, stack=st, name="rxb") for _ in range(4)])
        xT_r = Rot([k.sb([128, 8, 128], F32, stack=st, name="rxT") for _ in range(3)])
        sm_r = Rot([k.sb([128, 256], F32, stack=st, name="rsm") for _ in range(3)])
        oh_r = Rot([k.sb([128, 4, 32], F32, stack=st, name="roh") for _ in range(4)])
        di_r = Rot([k.sb([128, 4], I32, stack=st, name="rdi") for _ in range(3)])
        TT = {}

        RL = {}

        def rsl(ti):
            xt = xt_r.next()
            k.dma("sp", xt[:, :], X_in.ap[ti * 128:(ti + 1) * 128, :], reads=[X_in], writes=[xt])
            RL[ti] = xt

        def rs1(ti):
            xt = RL.pop(ti)
            ss = ss_r.next()
            rms_rstd(k, xt, xt[:, :], junk, ss, ss)
            xh = xh_r.next()
            xb = xb_r.next()
            k.op("act", lambda e: e.activation(out=xh[:, :], in_=xt[:, :], func=AF.Copy, scale=ss[:, 0:1]),
                 reads=[xt, ss], writes=[xh])
            k.op("dve", lambda e: e.tensor_copy(out=xb[:, :], in_=xh[:, :]), reads=[xh], writes=[xb])
            xT = xT_r.next()
            for hf in range(2):
                pt = g.psum.next()
                for q in range(4):
                    kc = hf * 4 + q
                    k.tr(pt, pt[:, q * 128:(q + 1) * 128], xh, xh[:, kc * 128:(kc + 1) * 128], cf, cf[:, C_ID:C_ID + 128])
                k.op("act" if hf == 0 else "dve",
                     (lambda e: e.copy(out=xT[:, 0:4, :], in_=pt[:, :].rearrange("p (q t) -> p q t", q=4))) if hf == 0 else
                     (lambda e: e.tensor_copy(out=xT[:, 4:8, :], in_=pt[:, :].rearrange("p (q t) -> p q t", q=4))),
                     reads=[pt], writes=[xT], join=(hf > 0))
            pl = g.psum.next()
            for kc in range(8):
                k.mm(pl, pl[:, 0:32], xT, xT[:, kc, :], Wr2, Wr2[:, kc, :], start=(kc == 0), stop=False)
            k.mm(pl, pl[:, 0:32], cf, cf[0:1, C_ONE:C_ONE + 128], br2, br2[0:1, :], start=False, stop=True)
            sm = sm_r.next()
            LG, T8, NT0, EX, SU, RS, MK, RKF, SF, DF = (sm[:, 0:32], sm[:, 32:40], sm[:, 40:41], sm[:, 41:45], sm[:, 45:46],
                                                        sm[:, 46:47], sm[:, 64:96], sm[:, 96:128], sm[:, 128:160], sm[:, 160:164])
            k.op("dve", lambda e: e.tensor_copy(out=LG, in_=pl[:, 0:32]), reads=[pl], writes=[sm])
            TT[ti] = (sm, xb)

        def rs2(ti):
            sm, xb = TT.pop(ti)
            LG, T8, NT0, EX, SU, RS, MK, RKF, SF, DF = (sm[:, 0:32], sm[:, 32:40], sm[:, 40:41], sm[:, 41:45], sm[:, 45:46],
                                                        sm[:, 46:47], sm[:, 64:96], sm[:, 96:128], sm[:, 128:160], sm[:, 160:164])
            k.op("dve", lambda e: e.max(out=T8, in_=LG), reads=[sm], writes=[sm])
            k.op("dve", lambda e: e.tensor_scalar(out=NT0, in0=sm[:, 32:33], scalar1=-1.0, scalar2=None, op0=ALU.mult),
                 reads=[sm], writes=[sm])
            k.op("act", lambda e: e.activation(out=EX, in_=sm[:, 32:36], func=AF.Exp, bias=NT0, scale=1.0, accum_out=SU),
                 reads=[sm], writes=[sm])
            k.op("dve", lambda e: e.reciprocal(out=RS, in_=SU), reads=[sm], writes=[sm])
            k.op("dve", lambda e: e.tensor_scalar(out=R.GATE[:, ti, :], in0=EX, scalar1=RS, scalar2=None, op0=ALU.mult),
                 reads=[sm], writes=[R.GATE])
            k.op("dve", lambda e: e.tensor_scalar(out=MK, in0=LG, scalar1=sm[:, 35:36], scalar2=None, op0=ALU.is_ge),
                 reads=[sm], writes=[sm])
            oh = oh_r.next()
            for kk in range(4):
                k.op("dve", lambda e: e.tensor_scalar(out=oh[:, kk, :], in0=LG, scalar1=sm[:, 32 + kk:33 + kk], scalar2=None,
                                                      op0=ALU.is_equal), reads=[sm], writes=[oh])
            pr = g.psum.next()
            k.mm(pr, pr[:, 0:32], cf, cf[:, C_US:C_US + 128], sm, MK, start=True, stop=True)
            k.mm(pr, pr[:, 32:64], cf, cf[:, C_ONE:C_ONE + 128], sm, MK, start=True, stop=True)
            k.op("dve", lambda e: e.tensor_tensor(out=RKF, in0=pr[:, 0:32], in1=carry[:, :], op=ALU.add),
                 reads=[pr, carry], writes=[sm])
            k.op("dve", lambda e: e.tensor_tensor(out=carry[:, :], in0=pr[:, 32:64], in1=carry[:, :], op=ALU.add),
                 reads=[pr, carry], writes=[carry])
            k.op("dve", lambda e: e.scalar_tensor_tensor(out=SF, in0=cf[:, C_IE:C_IE + 32], scalar=float(CAP), in1=RKF,
                                                         op0=ALU.mult, op1=ALU.add), reads=[cf, sm], writes=[sm])
            tmp = oh_r.next()
            for src, dst_ap, dst_t in ((SF, DF, sm), (RKF, R.RK[:, ti, :], R.RK), (cf[:, C_IE:C_IE + 32], R.EK[:, ti, :], R.EK)):
                k.op("dve", lambda e: e.tensor_tensor(out=tmp[:, :, :], in0=oh[:, :, :],
                                                      in1=src.unsqueeze(1).to_broadcast([128, 4, 32]), op=ALU.mult),
                     reads=[oh, sm, cf], writes=[tmp])
                k.op("dve", lambda e: e.tensor_reduce(out=dst_ap, in_=tmp[:, :, :], axis=AX.X, op=ALU.add),
                     reads=[tmp], writes=[dst_t])
            di = di_r.next()
            k.op("dve", lambda e: e.tensor_copy(out=di[:, :], in_=DF), reads=[sm], writes=[di])
            for kk in range(4):
                k.op("pool", lambda e: e.indirect_dma_start(out=BUF.ap[:, :], out_offset=IOA(di[:, kk:kk + 1]),
                                                            in_=xb[:, :], in_offset=None),
                     reads=[xb, di], writes=[], dma=True)

        rsl(0)
        rsl(1)
        rs1(0)
        for ti in range(NT):
            if ti + 2 < NT:
                rsl(ti + 2)
            if ti + 1 < NT:
                rs1(ti + 1)
            rs2(ti)
        t1 = k.sb([128, 32], F32, stack=st, name="t1")
        nb256 = k.sb([128, 32], F32, stack=st, name="nb256")
        bend = k.sb([128, 32], F32, stack=st, name="bend")
        b256 = k.sb([128, NB], F32, stack=st, name="b256")
        k.op("dve", lambda e: e.tensor_scalar(out=b256[:, :], in0=cf[:, C_IB:C_IB + NB], scalar1=256.0, scalar2=None,
                                              op0=ALU.mult), reads=[cf], writes=[b256])
        cmp = k.sb([128, 32, 16], F32, stack=st, name="cmp")
        k.op("dve", lambda e: e.tensor_tensor(out=cmp[:, :, :], in0=carry[:, :].unsqueeze(2).to_broadcast([128, 32, 16]),
                                              in1=b256[:, 0:16].unsqueeze(1).to_broadcast([128, 32, 16]), op=ALU.is_gt),
             reads=[carry, b256], writes=[cmp])
        k.op("dve", lambda e: e.tensor_reduce(out=t1[:, :], in_=cmp[:, :, :], axis=AX.X, op=ALU.add), reads=[cmp], writes=[t1])
        k.op("dve", lambda e: e.tensor_scalar(out=nb256[:, :], in0=t1[:, :], scalar1=256.0, scalar2=None, op0=ALU.mult),
             reads=[t1], writes=[nb256])
        k.op("dve", lambda e: e.tensor_copy(out=bend[:, 0:1], in_=nb256[:, 0:1]), reads=[nb256], writes=[bend])
        for e_ in range(1, 32):
            k.op("dve", lambda e: e.tensor_tensor(out=bend[:, e_:e_ + 1], in0=bend[:, e_ - 1:e_], in1=nb256[:, e_:e_ + 1],
                                                  op=ALU.add), reads=[bend, nb256], writes=[bend])
        k.op("dve", lambda e: e.tensor_tensor(out=R.BST[:, :], in0=bend[:, :], in1=nb256[:, :], op=ALU.subtract),
             reads=[bend, nb256], writes=[R.BST])
        big = k.sb([128, NB, 32], F32, stack=st, name="big")
        eb = k.sb([128, NB], F32, stack=st, name="eb")
        bst = k.sb([128, NB], F32, stack=st, name="bst")
        sbase = k.sb([128, NB], F32, stack=st, name="sbase")
        k.op("dve", lambda e: e.tensor_tensor(out=big[:, :, :], in0=bend[:, :].unsqueeze(1).to_broadcast([128, NB, 32]),
                                              in1=b256[:, :].unsqueeze(2).to_broadcast([128, NB, 32]), op=ALU.is_le),
             reads=[bend, b256], writes=[big])
        k.op("dve", lambda e: e.tensor_reduce(out=eb[:, :], in_=big[:, :, :], axis=AX.X, op=ALU.add), reads=[big], writes=[eb])
        k.op("dve", lambda e: e.tensor_tensor(out=big[:, :, :], in0=big[:, :, :],
                                              in1=bend[:, :].unsqueeze(1).to_broadcast([128, NB, 32]), op=ALU.mult),
             reads=[big, bend], writes=[big])
        k.op("dve", lambda e: e.tensor_reduce(out=bst[:, :], in_=big[:, :, :], axis=AX.X, op=ALU.max), reads=[big], writes=[bst])
        k.op("dve", lambda e: e.tensor_scalar(out=eb[:, :], in0=eb[:, :], scalar1=31.0, scalar2=None, op0=ALU.min),
             reads=[eb], writes=[eb])
        k.op("dve", lambda e: e.tensor_tensor(out=bst[:, :], in0=b256[:, :], in1=bst[:, :], op=ALU.subtract),
             reads=[b256, bst], writes=[bst])
        k.op("dve", lambda e: e.tensor_scalar(out=bst[:, :], in0=bst[:, :], scalar1=float(CAP - 256), scalar2=None, op0=ALU.min),
             reads=[bst], writes=[bst])
        k.op("dve", lambda e: e.scalar_tensor_tensor(out=sbase[:, :], in0=eb[:, :], scalar=float(CAP), in1=bst[:, :],
                                                     op0=ALU.mult, op1=ALU.add), reads=[eb, bst], writes=[sbase])
        tf = k.sb([128, NB, 8], F32, stack=st, name="tf")
        for h in range(2):
            k.op("dve", lambda e: e.tensor_scalar(out=tf[:, :, h], in0=sbase[:, :], scalar1=cf[:, C_IP:C_IP + 1],
                                                  scalar2=float(h * 128), op0=ALU.add, op1=ALU.add),
                 reads=[sbase, cf], writes=[tf])
        k.op("dve", lambda e: e.tensor_copy(out=R.IDT[:, :, :], in_=tf[:, :, 0:2]), reads=[tf], writes=[R.IDT])
        skp = k.sb([128, NB], F32, stack=st, name="skp")
        k.op("dve", lambda e: e.memset(skp[:, :], 0.0), writes=[skp])
        k.op("dve", lambda e: e.tensor_tensor(out=skp[:, 2:NB], in0=eb[:, 2:NB], in1=eb[:, 0:NB - 2], op=ALU.is_equal),
             reads=[eb, skp], writes=[skp])
        k.op("dve", lambda e: e.tensor_scalar(out=skp[:, :], in0=skp[:, :], scalar1=float(1 << 20), scalar2=None, op0=ALU.mult),
             reads=[skp], writes=[skp])
        for kc in range(8):
            k.op("dve", lambda e: e.tensor_scalar(out=tf[:, :, kc], in0=eb[:, :], scalar1=1024.0,
                                                  scalar2=float(l * 32 * 1024 + kc * 128), op0=ALU.mult, op1=ALU.add),
                 reads=[eb], writes=[tf])
        k.op("dve", lambda e: e.tensor_scalar(out=tf[:, :, :], in0=tf[:, :, :], scalar1=cf[:, C_IP:C_IP + 1], scalar2=None,
                                              op0=ALU.add), reads=[tf, cf], writes=[tf])
        k.op("dve", lambda e: e.tensor_tensor(out=tf[:, :, :], in0=tf[:, :, :], in1=skp[:, :].unsqueeze(2).to_broadcast([128, NB, 8]),
                                              op=ALU.add), reads=[tf, skp], writes=[tf])
        k.op("dve", lambda e: e.tensor_copy(out=R.IDW[:, :, :], in_=tf[:, :, :]), reads=[tf], writes=[R.IDW])
        k.op("dve", lambda e: e.scalar_tensor_tensor(out=tf[:, :, 0], in0=eb[:, :], scalar=float(l * 32), in1=skp[:, :],
                                                     op0=ALU.add, op1=ALU.add), reads=[eb, skp], writes=[tf])
        k.op("dve", lambda e: e.tensor_copy(out=R.IDB[:, :], in_=tf[:, :, 0]), reads=[tf], writes=[R.IDB])
        k.op("dve", lambda e: e.tensor_scalar(out=tf[:, :, 1], in0=tf[:, :, 0], scalar1=128.0, scalar2=cf[:, C_IP:C_IP + 1],
                                              op0=ALU.mult, op1=ALU.add), reads=[tf, cf], writes=[tf])
        k.op("dve", lambda e: e.tensor_copy(out=R.IDC[:, :], in_=tf[:, :, 1]), reads=[tf], writes=[R.IDC])
        ekf = R.EK[:, :, :].rearrange("p t k -> p (t k)")
        big2 = k.sb([128, 128, 32], F32, stack=st, name="big2")
        cif = k.sb([128, 128], F32, stack=st, name="cif")
        k.op("dve", lambda e: e.tensor_tensor(out=big2[:, :, :], in0=cf[:, C_IE:C_IE + 32].unsqueeze(1).to_broadcast([128, 128, 32]),
                                              in1=ekf.unsqueeze(2).to_broadcast([128, 128, 32]), op=ALU.is_equal),
             reads=[cf, R.EK], writes=[big2])
        k.op("dve", lambda e: e.tensor_tensor(out=big2[:, :, :], in0=big2[:, :, :],
                                              in1=R.BST[:, :].unsqueeze(1).to_broadcast([128, 128, 32]), op=ALU.mult),
             reads=[big2, R.BST], writes=[big2])
        k.op("dve", lambda e: e.tensor_reduce(out=cif[:, :], in_=big2[:, :, :], axis=AX.X, op=ALU.add), reads=[big2], writes=[cif])
        k.op("dve", lambda e: e.tensor_tensor(out=cif[:, :], in0=cif[:, :], in1=R.RK[:, :, :].rearrange("p t k -> p (t k)"),
                                              op=ALU.add), reads=[cif, R.RK], writes=[cif])
        k.op("dve", lambda e: e.tensor_copy(out=R.CIDX[:, :, :].rearrange("p t k -> p (t k)"), in_=cif[:, :]),
             reads=[cif], writes=[R.CIDX])
        k.barrier()


def cast_weights(k, io, l, W1B, W2B):
    for e_ in range(32):
        r0 = (l * 32 + e_) * 1024
        k.dma("pool", W1B.ap[e_ * 1024:(e_ + 1) * 1024, :], io["w1"][r0:r0 + 1024, :], writes=[W1B], join=(e_ > 0))
        k.dma("pool", W2B.ap[e_ * 1024:(e_ + 1) * 1024, :], io["w2"][r0:r0 + 1024, :], writes=[W2B], join=(e_ > 0))


class RState:
    def __init__(self, k):
        self.GATE = k.sb([128, NT, 4], F32, name="GATE")
        self.RK = k.sb([128, NT, 4], F32, name="RK")
        self.EK = k.sb([128, NT, 4], F32, name="EK")
        self.BST = k.sb([128, 32], F32, name="BST")
        self.IDT = k.sb([128, NB, 2], I32, name="IDT")
        self.IDW = k.sb([128, NB, 8], I32, name="IDW")
        self.IDB = k.sb([128, NB], I32, name="IDB")
        self.IDC = k.sb([128, NB], I32, name="IDC")
        self.CIDX = k.sb([128, NT, 4], I32, name="CIDX")


def expert_phase(k, io, g, l, BUF, Y, R, W1B, W2B, nblocks=NB):
    cf, cb = g.cf, g.cb
    n = 1 if l == 0 else 4
    Bt, Bc = g.Bsh[n]
    with contextlib.ExitStack() as st:
        w1_r = Rot([k.sb([128, 8, 2048], BF16, stack=st, name="w1e") for _ in range(2)])
        w2_r = Rot([k.sb([128, 8, 1024], BF16, stack=st, name="w2e") for _ in range(2)])
        b1_r = Rot([k.sb([128, 16], F32, stack=st, name="b1c") for _ in range(2)])
        b2_r = Rot([k.sb([2, 1024], BF16, stack=st, name="b2r") for _ in range(2)])
        xb_r = Rot([k.sb([128, 2, 1024], BF16, stack=st, name="exb") for _ in range(2)])
        xT_r = Rot([k.sb([128, 8, 256], BF16, stack=st, name="exT") for _ in range(2)])
        gl_r = Rot([k.sb([128, 256], F32, stack=st, name="gl") for _ in range(3)])
        sg_r = Rot([k.sb([128, 256], F32, stack=st, name="sg") for _ in range(3)])
        ll_r = Rot([k.sb([128, 256], F32, stack=st, name="ll") for _ in range(3)])
        aT_r = Rot([k.sb([128, 8, 256], BF16, stack=st, name="aT") for _ in range(2)])
        yo_r = Rot([k.sb([128, 2, 1024], F32, stack=st, name="yo") for _ in range(2)])
        psb_r = Rot([g.psb, g.psb2])
        ones_bf = cb[0:1, B_ONE:B_ONE + 128]
        blk = {}

        def load(b):
            d = dict(xb=xb_r.next(), w1e=w1_r.next(), w2e=w2_r.next(), b1r=b1_r.next(), b2r=b2_r.next())
            for h in range(2):
                k.op("pool", lambda e: e.indirect_dma_start(out=d["xb"][:, h, :], out_offset=None, in_=BUF.ap[:, :],
                                                            in_offset=IOA(R.IDT[:, b, h:h + 1])),
                     reads=[R.IDT], writes=[d["xb"]], dma=True, join=(h > 0))
            for kc in range(8):
                k.op("pool", lambda e: e.indirect_dma_start(out=d["w1e"][:, kc, :], out_offset=None, in_=W1B.ap[:, :],
                                                            in_offset=IOA(R.IDW[:, b, kc:kc + 1]), bounds_check=g.bc_w, oob_is_err=False),
                     reads=[R.IDW, W1B], writes=[d["w1e"]], dma=True, join=(kc > 0))
            k.op("pool", lambda e: e.indirect_dma_start(out=d["b1r"][:, :], out_offset=None, in_=io["b1"][:, :],
                                                        in_offset=IOA(R.IDC[:, b:b + 1]), bounds_check=g.bc_c, oob_is_err=False),
                 reads=[R.IDC], writes=[d["b1r"]], dma=True)
            for kc in range(8):
                k.op("pool", lambda e: e.indirect_dma_start(out=d["w2e"][:, kc, :], out_offset=None, in_=W2B.ap[:, :],
                                                            in_offset=IOA(R.IDW[:, b, kc:kc + 1]), bounds_check=g.bc_w, oob_is_err=False),
                     reads=[R.IDW, W2B], writes=[d["w2e"]], dma=True, join=(kc > 0))
            k.op("pool", lambda e: e.indirect_dma_start(out=d["b2r"][0:2, :], out_offset=None, in_=io["b2"][:, :],
                                                        in_offset=IOA(R.IDB[0:2, b:b + 1]), bounds_check=g.bc_b, oob_is_err=False),
                 reads=[R.IDB], writes=[d["b2r"]], dma=True)
            blk[b] = d

        def transp(b):
            d = blk[b]
            xb = d["xb"]
            xT = xT_r.next()
            d["xT"] = xT
            first = True
            for h in range(2):
                pt = psb_r.next()
                for kc in range(8):
                    k.tr(pt, pt[:, kc * 128:(kc + 1) * 128], xb, xb[:, h, kc * 128:(kc + 1) * 128], cb, cb[:, B_ID:B_ID + 128])
                for kc in range(8):
                    if h == 0:
                        k.op("dve", lambda e: e.tensor_scalar(out=xT[:, kc, h * 128:(h + 1) * 128], in0=pt[:, kc * 128:(kc + 1) * 128],
                                                              scalar1=g.A[:, n, kc:kc + 1], scalar2=Bt[:, Bc + kc:Bc + kc + 1],
                                                              op0=ALU.mult, op1=ALU.add), reads=[pt, g.A, Bt], writes=[xT],
                             join=not first)
                    else:
                        k.op("act", lambda e: e.activation(out=xT[:, kc, h * 128:(h + 1) * 128], in_=pt[:, kc * 128:(kc + 1) * 128],
                                                           func=AF.Identity, scale=g.A[:, n, kc:kc + 1],
                                                           bias=Bt[:, Bc + kc:Bc + kc + 1]), reads=[pt, g.A, Bt], writes=[xT],
                             join=not first)
                    first = False

        def h1(b):
            d = blk[b]
            w1e, b1c, xT = d["w1e"], d["b1r"], d["xT"]
            aT = aT_r.next()
            d["aT"] = aT
            for j2 in range(4):
                pg_ = g.psum.next()
                pl_ = g.psum.next()
                for q in range(2):
                    j = 2 * j2 + q
                    for bank, fo in ((pg_, j), (pl_, 8 + j)):
                        o = bank[:, q * 256:(q + 1) * 256]
                        for kc in range(8):
                            k.mm(bank, o, w1e, w1e[:, kc, fo * 128:(fo + 1) * 128], xT, xT[:, kc, :], start=(kc == 0), stop=(kc == 7))
                for q in range(2):
                    j = 2 * j2 + q
                    gl = gl_r.next()
                    sg = sg_r.next()
                    ll = ll_r.next()
                    k.op("dve", lambda e: e.tensor_scalar(out=gl[:, :], in0=pg_[:, q * 256:(q + 1) * 256], scalar1=b1c[:, j:j + 1],
                                                          scalar2=7.0, op0=ALU.add, op1=ALU.min), reads=[pg_, b1c], writes=[gl])
                    k.op("act", lambda e: e.activation(out=ll[:, :], in_=pl_[:, q * 256:(q + 1) * 256], func=AF.Identity,
                                                       bias=b1c[:, 8 + j:9 + j], scale=1.0), reads=[pl_, b1c], writes=[ll])
                    k.op("act", lambda e: e.activation(out=sg[:, :], in_=gl[:, :], func=AF.Sigmoid, scale=1.702),
                         reads=[gl], writes=[sg])
                    k.op("dve", lambda e: e.tensor_scalar(out=ll[:, :], in0=ll[:, :], scalar1=7.0, scalar2=-7.0,
                                                          op0=ALU.min, op1=ALU.max), reads=[ll], writes=[ll])
                    k.op("dve", lambda e: e.tensor_tensor(out=gl[:, :], in0=gl[:, :], in1=sg[:, :], op=ALU.mult),
                         reads=[gl, sg], writes=[gl])
                    k.op("dve", lambda e: e.scalar_tensor_tensor(out=aT[:, j, :], in0=ll[:, :], scalar=1.0, in1=gl[:, :],
                                                                 op0=ALU.add, op1=ALU.mult), reads=[ll, gl], writes=[aT], join=(j > 0))

        def yout(b):
            d = blk[b]
            aT, w2e, b2r = d["aT"], d["w2e"], d["b2r"]
            yo = yo_r.next()
            i = 0
            for h in range(2):
                for nb in range(2):
                    py = g.psum.next()
                    for fo in range(8):
                        k.mm(py, py[:, :], aT, aT[:, fo, h * 128:(h + 1) * 128], w2e, w2e[:, fo, nb * 512:(nb + 1) * 512],
                             start=(fo == 0), stop=False)
                    k.mm(py, py[:, :], cb, ones_bf, b2r, b2r[0:1, nb * 512:(nb + 1) * 512], start=False, stop=True)
                    k.op("act", lambda e: e.copy(out=yo[:, h, nb * 512:(nb + 1) * 512], in_=py[:, :]), reads=[py], writes=[yo],
                         join=(i > 0))
                    i += 1
            k.dma("sp", Y.ap[b * 256:(b + 1) * 256, :].rearrange("(h p) d -> p h d", p=128), yo[:, :, :], reads=[yo], writes=[])
            del blk[b]

        load(0)
        transp(0)
        for b in range(nblocks):
            if b + 1 < nblocks:
                load(b + 1)
            h1(b)
            if b + 1 < nblocks:
                transp(b + 1)
            yout(b)
        k.barrier()


def combine_phase(k, io, g, l, X_in, Y, X_out, R, final=False):
    cf = g.cf
    gate = g.gates[l][1]
    with contextlib.ExitStack() as st:
        xt_r = Rot([k.sb([128, 1024], F32, stack=st, name="cxt") for _ in range(4)])
        yk_r = Rot([k.sb([128, 4, 1024], F32, stack=st, name="cyk") for _ in range(4)])
        acc_r = Rot([k.sb([128, 1024], F32, stack=st, name="cacc") for _ in range(3)])
        if final:
            fg = k.sb([128, 1024], F32, stack=st, name="fg")
            k.dma("sp", fg[:, :], io["fing"][0:1, :].to_broadcast([128, 1024]), writes=[fg])
            junk = k.sb([128, 1024], BF16, stack=st, name="cjunk")
            ss_r = Rot([k.sb([128, 2], F32, stack=st, name="css") for _ in range(2)])
        CL = {}

        def cl(ti):
            xt = xt_r.next()
            k.dma("sp", xt[:, :], X_in.ap[ti * 128:(ti + 1) * 128, :], reads=[X_in], writes=[xt])
            yk = yk_r.next()
            for kk in range(4):
                k.op("pool", lambda e: e.indirect_dma_start(out=yk[:, kk, :], out_offset=None, in_=Y.ap[:, :],
                                                            in_offset=IOA(R.CIDX[:, ti, kk:kk + 1])),
                     reads=[R.CIDX], writes=[yk], dma=True, join=(kk > 0))
            CL[ti] = (xt, yk)

        cl(0)
        cl(1)
        for ti in range(NT):
            if ti + 2 < NT:
                cl(ti + 2)
            xt, yk = CL.pop(ti)
            acc = acc_r.next()
            k.op("dve", lambda e: e.tensor_scalar(out=acc[:, :], in0=yk[:, 0, :], scalar1=R.GATE[:, ti, 0:1], scalar2=None,
                                                  op0=ALU.mult), reads=[yk, R.GATE], writes=[acc])
            for kk in range(1, 4):
                k.op("dve", lambda e: e.scalar_tensor_tensor(out=acc[:, :], in0=yk[:, kk, :], scalar=R.GATE[:, ti, kk:kk + 1],
                                                             in1=acc[:, :], op0=ALU.mult, op1=ALU.add),
                     reads=[yk, R.GATE, acc], writes=[acc])
            k.op("dve", lambda e: e.tensor_tensor(out=acc[:, :], in0=acc[:, :], in1=gate[:, :], op=ALU.mult),
                 reads=[acc, gate], writes=[acc])
            k.op("dve", lambda e: e.tensor_tensor(out=acc[:, :], in0=acc[:, :], in1=xt[:, :], op=ALU.add),
                 reads=[acc, xt], writes=[acc])
            if final:
                ss = ss_r.next()
                rms_rstd(k, acc, acc[:, :], junk, ss, ss)
                k.op("dve", lambda e: e.scalar_tensor_tensor(out=acc[:, :], in0=acc[:, :], scalar=ss[:, 0:1], in1=fg[:, :],
                                                             op0=ALU.mult, op1=ALU.mult), reads=[acc, ss, fg], writes=[acc])
            k.dma("sp", X_out.ap[ti * 128:(ti + 1) * 128, :], acc[:, :], reads=[acc], writes=[])
        k.barrier()


NH = 16
DH = 64
LVL = 9


def head_rmsnorm(k, src_ps_list, dst_bf, wk, gb, st_tiles):
    raw, sq, hs = st_tiles
    for nb, ps in enumerate(src_ps_list):
        k.op("act", lambda e: e.copy(out=raw[:, nb * 512:(nb + 1) * 512], in_=ps[:, :]), reads=[ps], writes=[raw], join=(nb > 0))
    k.op("dve", lambda e: e.tensor_tensor(out=sq[:, :], in0=raw[:, :], in1=raw[:, :], op=ALU.mult), reads=[raw], writes=[sq])
    k.op("dve", lambda e: e.tensor_reduce(out=hs[:, 0:16], in_=sq[:, :].rearrange("p (h d) -> p h d", h=NH), axis=AX.X, op=ALU.add),
         reads=[sq], writes=[hs])
    k.op("dve", lambda e: e.tensor_scalar(out=hs[:, 0:16], in0=hs[:, 0:16], scalar1=1.0 / DH, scalar2=EPS, op0=ALU.mult, op1=ALU.add),
         reads=[hs], writes=[hs])
    k.op("act", lambda e: e.activation(out=hs[:, 0:16], in_=hs[:, 0:16], func=AF.Sqrt), reads=[hs], writes=[hs])
    k.op("dve", lambda e: e.reciprocal(out=hs[:, 0:16], in_=hs[:, 0:16]), reads=[hs], writes=[hs])
    k.op("dve", lambda e: e.tensor_tensor(out=sq[:, :].rearrange("p (h d) -> p h d", h=NH),
                                          in0=raw[:, :].rearrange("p (h d) -> p h d", h=NH),
                                          in1=hs[:, 0:16].unsqueeze(2).to_broadcast([128, NH, DH]), op=ALU.mult),
         reads=[raw, hs], writes=[sq])
    k.op("pool", lambda e: e.tensor_tensor(out=dst_bf[:, :].rearrange("p (h d) -> p h d", h=NH),
                                           in0=sq[:, :].rearrange("p (h d) -> p h d", h=NH),
                                           in1=gb[:, :].unsqueeze(1).to_broadcast([128, NH, DH]), op=ALU.mult),
         reads=[sq, gb], writes=[dst_bf])


def kvq_phase(k, io, g, X_in, KT, QT, GT, V, FP, FPT, after_loads=None):
    cf, cb = g.cf, g.cb
    with contextlib.ExitStack() as st:
        kvw = k.sb([128, 8, 2064], BF16, stack=st, name="kvw")
        wqg = k.sb([128, 8, 2048], BF16, stack=st, name="wqg")
        for kc in range(8):
            k.dma("pool", kvw[:, kc, :], io["kvw"][kc * 128:(kc + 1) * 128, :], writes=[kvw])
            k.dma("pool", wqg[:, kc, :], io["wqg"][kc * 128:(kc + 1) * 128, :], writes=[wqg])
        if after_loads is not None:
            after_loads()
        kgb = k.sb([128, 64], F32, stack=st, name="kgb")
        qgb = k.sb([128, 64], F32, stack=st, name="qgb")
        fgb = k.sb([128, 16], F32, stack=st, name="fgb")
        k.dma("sp", kgb[:, :], io["kng"][0:1, :].to_broadcast([128, 64]), writes=[kgb])
        k.dma("sp", qgb[:, :], io["qng"][0:1, :].to_broadcast([128, 64]), writes=[qgb])
        k.dma("sp", fgb[:, :], io["fgb"][0:1, :].to_broadcast([128, 16]), writes=[fgb])
        carry = k.sb([128, 16], F32, stack=st, name="fcarry")
        k.op("dve", lambda e: e.memset(carry[:, :], 0.0), writes=[carry])
        xt_r = Rot([k.sb([128, 1024], F32, stack=st, name="axt") for _ in range(4)])
        junk = k.sb([128, 1024], BF16, stack=st, name="ajunk")
        ss_r = Rot([k.sb([128, 2], F32, stack=st, name="ass") for _ in range(2)])
        xh_r = Rot([k.sb([128, 1024], BF16, stack=st, name="axh") for _ in range(2)])
        hk_r = Rot([k.sb([128, 8, 128], BF16, stack=st, name="hk") for _ in range(2)])
        hq_r = Rot([k.sb([128, 8, 128], BF16, stack=st, name="hq") for _ in range(2)])
        raw = k.sb([128, 1024], F32, stack=st, name="raw")
        sq = k.sb([128, 1024], F32, stack=st, name="sq")
        hs = k.sb([128, 64], F32, stack=st, name="hs")
        kn_r = Rot([k.sb([128, 1024], BF16, stack=st, name="kn") for _ in range(2)])
        vb_r = Rot([k.sb([128, 1024], BF16, stack=st, name="vb") for _ in range(2)])
        tT_r = Rot([k.sb([128, 8, 128], BF16, stack=st, name="tT") for _ in range(3)])
        gT_r = Rot([k.sb([128, 8, 128], BF16, stack=st, name="agT") for _ in range(2)])
        fpt_r = Rot([k.sb([16, 128], F32, stack=st, name="fpt") for _ in range(2)])
        fz_r = Rot([k.sb([128, 48], F32, stack=st, name="fz") for _ in range(2)])
        A = g.A
        Btk, Bck = g.Bsh[2]
        Btq, Bcq = g.Bsh[3]
        psb_r = Rot([g.psb, g.psb2])
        raw2 = k.sb([128, 1024], F32, stack=st, name="raw2")
        sq2 = k.sb([128, 1024], F32, stack=st, name="sq2")
        hs2 = k.sb([128, 64], F32, stack=st, name="hs2")
        T = {}

        XL = {}

        def stL(ti):
            xt = xt_r.next()
            k.dma("sp", xt[:, :], X_in.ap[ti * 128:(ti + 1) * 128, :], reads=[X_in], writes=[xt])
            XL[ti] = xt

        def stA(ti):
            xt = XL.pop(ti)
            ss = ss_r.next()
            rms_rstd(k, xt, xt[:, :], junk, ss, ss)
            xh = xh_r.next()
            k.op("act", lambda e: e.activation(out=xh[:, :], in_=xt[:, :], func=AF.Copy, scale=ss[:, 0:1]),
                 reads=[xt, ss], writes=[xh])
            pt = psb_r.next()
            for kc in range(8):
                k.tr(pt, pt[:, kc * 128:(kc + 1) * 128], xh, xh[:, kc * 128:(kc + 1) * 128], cb, cb[:, B_ID:B_ID + 128])
            hk = hk_r.next()
            hq = hq_r.next()
            for kc in range(8):
                k.op("dve", lambda e: e.tensor_scalar(out=hk[:, kc, :], in0=pt[:, kc * 128:(kc + 1) * 128],
                                                      scalar1=A[:, 2, kc:kc + 1], scalar2=Btk[:, Bck + kc:Bck + kc + 1],
                                                      op0=ALU.mult, op1=ALU.add), reads=[pt, A, Btk], writes=[hk], join=(kc > 0))
            for kc in range(8):
                k.op("act", lambda e: e.activation(out=hq[:, kc, :], in_=pt[:, kc * 128:(kc + 1) * 128], func=AF.Identity,
                                                   scale=A[:, 3, kc:kc + 1], bias=Btq[:, Bcq + kc:Bcq + kc + 1]),
                     reads=[pt, A, Btq, hk], writes=[hq], join=(kc > 0))
            T[ti] = dict(hk=hk, hq=hq)

        def stB(ti):
            d = T[ti]
            hk, hq = d["hk"], d["hq"]
            pks = []
            for nb in range(2):
                pk = g.psum.next()
                for kc in range(8):
                    k.mm(pk, pk[:, :], hk, hk[:, kc, :], kvw, kvw[:, kc, nb * 512:(nb + 1) * 512], start=(kc == 0), stop=(kc == 7))
                pks.append(pk)
            kn = kn_r.next()
            head_rmsnorm(k, pks, kn, None, kgb, (raw, sq, hs))
            pqs = []
            for nb in range(2):
                pq = g.psum.next()
                for kc in range(8):
                    k.mm(pq, pq[:, :], hq, hq[:, kc, :], wqg, wqg[:, kc, nb * 512:(nb + 1) * 512], start=(kc == 0), stop=(kc == 7))
                pqs.append(pq)
            qn = kn_r.next()
            head_rmsnorm(k, pqs, qn, None, qgb, (raw2, sq2, hs2))
            vb = vb_r.next()
            for nb in range(2):
                pv = g.psum.next()
                for kc in range(8):
                    k.mm(pv, pv[:, :], hk, hk[:, kc, :], kvw, kvw[:, kc, 1024 + nb * 512:1024 + (nb + 1) * 512],
                         start=(kc == 0), stop=(kc == 7))
                k.op("act", lambda e: e.copy(out=vb[:, nb * 512:(nb + 1) * 512], in_=pv[:, :]), reads=[pv], writes=[vb], join=(nb > 0))
            k.dma("act", V.ap[ti * 128:(ti + 1) * 128, :], vb[:, :], reads=[vb], writes=[])
            pf = g.psum.next()
            for kc in range(8):
                k.mm(pf, pf[:, 0:16], hk, hk[:, kc, :], kvw, kvw[:, kc, 2048:2064], start=(kc == 0), stop=(kc == 7))
            fz = fz_r.next()
            k.op("dve", lambda e: e.tensor_tensor(out=fz[:, 16:32], in0=pf[:, 0:16], in1=fgb[:, :], op=ALU.add),
                 reads=[pf, fgb], writes=[fz])
            k.op("act", lambda e: e.activation(out=fz[:, 16:32], in_=fz[:, 16:32], func=AF.Exp, scale=-1.0), reads=[fz], writes=[fz])
            k.op("act", lambda e: e.activation(out=fz[:, 32:48], in_=fz[:, 16:32], func=AF.Ln, bias=1.0, scale=1.0),
                 reads=[fz], writes=[fz])
            gT = gT_r.next()
            for c4 in range(2):
                pg = g.psum.next()
                for q in range(4):
                    c = c4 * 4 + q
                    for kc in range(8):
                        k.mm(pg, pg[:, q * 128:(q + 1) * 128], wqg, wqg[:, kc, 1024 + c * 128:1024 + (c + 1) * 128], hq, hq[:, kc, :],
                             start=(kc == 0), stop=(kc == 7))
                k.op("act", lambda e: e.activation(out=gT[:, c4 * 4:(c4 + 1) * 4, :], in_=pg[:, :].rearrange("p (q t) -> p q t", q=4),
                                                   func=AF.Sigmoid), reads=[pg], writes=[gT], join=(c4 > 0))
            k.dma("act", GT.ap[:, ti * 128:(ti + 1) * 128].rearrange("(c p) t -> p c t", p=128), gT[:, :, :], reads=[gT], writes=[])
            pc = g.psum.next()
            k.mm(pc, pc[:, 0:16], cf, cf[:, C_U:C_U + 128], fz, fz[:, 32:48], start=True, stop=True)
            k.mm(pc, pc[:, 16:32], cf, cf[:, C_ONE:C_ONE + 128], fz, fz[:, 32:48], start=True, stop=True)
            k.op("dve", lambda e: e.tensor_tensor(out=FP[:, ti, :], in0=pc[:, 0:16], in1=carry[:, :], op=ALU.add),
                 reads=[pc, carry], writes=[FP])
            k.op("dve", lambda e: e.tensor_tensor(out=carry[:, :], in0=pc[:, 16:32], in1=carry[:, :], op=ALU.add),
                 reads=[pc, carry], writes=[carry])
            pft = g.psum.next()
            k.tr(pft, pft[0:16, 0:128], FP, FP[:, ti, :], cf, cf[:, C_ID:C_ID + 128])
            fpt = fpt_r.next()
            k.op("dve", lambda e: e.tensor_copy(out=fpt[0:16, :], in_=pft[0:16, 0:128]), reads=[pft], writes=[fpt])
            k.dma("sp", FPT.ap[:, ti * 128:(ti + 1) * 128], fpt[0:16, :], reads=[fpt], writes=[])
            for src, dst in ((kn, KT), (qn, QT)):
                pt2 = psb_r.next()
                for c in range(8):
                    k.tr(pt2, pt2[:, c * 128:(c + 1) * 128], src, src[:, c * 128:(c + 1) * 128], cb, cb[:, B_ID:B_ID + 128])
                tT = tT_r.next()
                k.op("act", lambda e: e.copy(out=tT[:, :, :], in_=pt2[:, :].rearrange("p (c t) -> p c t", c=8)), reads=[pt2], writes=[tT])
                k.dma("sp", dst.ap[:, ti * 128:(ti + 1) * 128].rearrange("(c p) t -> p c t", p=128), tT[:, :, :], reads=[tT], writes=[])
            del T[ti]

        stL(0)
        stL(1)
        stA(0)
        for ti in range(NT):
            if ti + 2 < NT:
                stL(ti + 2)
            if ti + 1 < NT:
                stA(ti + 1)
            stB(ti)
        k.barrier()


def attn_phase(k, io, g, KT, QT, GT, V, FP, FPT, OT, heads=range(NH)):
    cf, cb = g.cf, g.cb
    QW = 512
    NQ = S // QW
    banks = g.psum.tiles
    sbanks = [banks[0], banks[1], banks[2]]
    extra = []
    for pb in (g.psb, g.psb2):
        t = Tk(pb.ap.bitcast(F32), pb.name + "_f32")
        extra.append(t)
    ps_s = Rot(sbanks + extra)
    ps_o = Rot(banks[3:5])
    ps_b = banks[5]
    LOOK = 4
    with contextlib.ExitStack() as st:
        kT_r = Rot([k.sb([65, S], BF16, stack=st, name="kTh") for _ in range(3)])
        qT_r = Rot([k.sb([65, S], BF16, stack=st, name="qTh") for _ in range(3)])
        fr = k.sb([65, S], F32, stack=st, name="fr")
        fr2 = k.sb([65, S], F32, stack=st, name="fr2")
        for kt_ in kT_r.tiles:
            k.op("pool", lambda e: e.memset(kt_[64:65, :], 1.0), writes=[kt_])
        gT_r = Rot([k.sb([64, S], BF16, stack=st, name="gTh") for _ in range(3)])
        Vh_r = Rot([k.sb([128, NT, 65], BF16, stack=st, name="Vh") for _ in range(3)])
        oT_r = Rot([k.sb([64, S], BF16, stack=st, name="oTh") for _ in range(3)])
        for vt in Vh_r.tiles:
            k.op("pool", lambda e: e.memset(vt[:, :, 64:65], 1.0), writes=[vt])
        Bq_r = Rot([k.sb([128, NQ, NT], F32, stack=st, name="Bq") for _ in range(3)])
        frb = k.sb([128, NQ], F32, stack=st, name="frb")
        pT_r = Rot([k.sb([128, QW], BF16, stack=st, name="pT") for _ in range(LOOK + 2)])
        rl_r = Rot([k.sb([128, QW], F32, stack=st, name="rl") for _ in range(2)])
        o1_r = Rot([k.sb([64, QW], F32, stack=st, name="o1") for _ in range(2)])
        HL = {}

        def hload(h):
            kT = kT_r.next()
            qT = qT_r.next()
            gT = gT_r.next()
            Vh = Vh_r.next()
            oT = oT_r.next()
            k.dma("sp", kT[0:64, :], KT.ap[h * 64:(h + 1) * 64, :], writes=[kT])
            k.dma("sp", qT[0:64, :], QT.ap[h * 64:(h + 1) * 64, :], writes=[qT])
            k.dma("sp", fr[64:65, :], FPT.ap[h:h + 1, :], writes=[fr])
            k.op("dve", lambda e: e.tensor_tensor(out=fr2[64:65, :].rearrange("p (q w) -> p q w", w=QW),
                                                  in0=fr[64:65, :].rearrange("p (q w) -> p q w", w=QW)[:, :, QW - 1:QW].to_broadcast([1, NQ, QW]),
                                                  in1=fr[64:65, :].rearrange("p (q w) -> p q w", w=QW), op=ALU.subtract),
                 reads=[fr], writes=[fr2])
            k.op("dve", lambda e: e.tensor_scalar(out=qT[64:65, :], in0=fr2[64:65, :], scalar1=8.0, scalar2=None, op0=ALU.mult),
                 reads=[fr2], writes=[qT], join=True)
            k.dma("sp", gT[:, :], GT.ap[h * 64:(h + 1) * 64, :], writes=[gT])
            k.dma("sp", Vh[:, :, 0:64], V.ap[:, h * 64:(h + 1) * 64].rearrange("(kb p) d -> p kb d", p=128), writes=[Vh])
            k.mm(ps_b, ps_b[:, 0:NQ], cf, cf[:, C_S127:C_S127 + 128], FP, FP[:, :, h].rearrange("p (a b) -> p a b", b=4)[:, :, 3],
                 start=True, stop=True)
            k.op("dve", lambda e: e.tensor_copy(out=frb[:, :], in_=ps_b[:, 0:NQ]), reads=[ps_b], writes=[frb])
            Bq = Bq_r.next()
            k.op("dve", lambda e: e.tensor_tensor(out=Bq[:, :, :], in0=FP[:, :, h].unsqueeze(1).to_broadcast([128, NQ, NT]),
                                                  in1=frb[:, :].unsqueeze(2).to_broadcast([128, NQ, NT]), op=ALU.subtract),
                 reads=[FP, frb], writes=[Bq])
            HL[h] = (kT, qT, gT, Vh, oT, Bq)

        heads = list(heads)
        hload(heads[0])
        for hi, h in enumerate(heads):
            kT, qT, gT, Vh, oT, Bq = HL.pop(h)
            if hi + 1 < len(heads):
                hload(heads[hi + 1])
            pairs = [(qb, kb) for qb in range(NQ) for kb in range(4 * qb + 4)]
            state = {}
            deferred = []

            def emit_qk(i):
                qb, kb = pairs[i]
                nkb = 4 * qb + 4
                pS = ps_s.next()
                diag = kb >= nkb - 4
                k.mm(pS, pS[:, 0:QW], kT, kT[0:65, kb * 128:(kb + 1) * 128], qT, qT[0:65, qb * QW:(qb + 1) * QW],
                     start=True, stop=not diag)
                if diag:
                    m0 = B_NM + 512 * (kb - (nkb - 4))
                    k.mm(pS, pS[:, 0:QW], cb, cb[:, B_NI:B_NI + 128], cb, cb[:, m0:m0 + QW], start=False, stop=True)
                pT = pT_r.next()
                k.op("act", lambda e: e.activation(out=pT[:, :], in_=pS[:, 0:QW], func=AF.Exp, scale=0.125,
                                                   bias=Bq[:, qb, kb:kb + 1]), reads=[pS, Bq], writes=[pT])
                state[i] = pT

            def emit_pv(i):
                qb, kb = pairs[i]
                nkb = 4 * qb + 4
                if kb == 0:
                    state["po"] = ps_o.next()
                po = state["po"]
                pT = state.pop(i)
                k.mm(po, po[0:65, 0:QW], Vh, Vh[:, kb, 0:65], pT, pT[:, :], start=(kb == 0), stop=(kb == nkb - 1))
                if kb == nkb - 1:
                    rl = rl_r.next()
                    o1 = o1_r.next()
                    k.op("dve", lambda e: e.reciprocal(out=rl[64:65, :], in_=po[64:65, 0:QW]), reads=[po], writes=[rl])
                    k.op("dve", lambda e: e.tensor_copy(out=o1[:, :], in_=po[0:64, 0:QW]), reads=[po], writes=[o1])

                    def fin(rl=rl, o1=o1, qb=qb):
                        k.mm(ps_b, ps_b[0:64, 0:QW], cf, cf[64:65, C_ONE:C_ONE + 64], rl, rl[64:65, :], start=True, stop=True)
                        k.op("dve", lambda e: e.tensor_tensor(out=o1[:, :], in0=o1[:, :], in1=ps_b[0:64, 0:QW], op=ALU.mult),
                             reads=[o1, ps_b], writes=[o1])
                        k.op("pool", lambda e: e.tensor_tensor(out=oT[:, qb * QW:(qb + 1) * QW], in0=o1[:, :],
                                                               in1=gT[:, qb * QW:(qb + 1) * QW], op=ALU.mult),
                             reads=[o1, gT], writes=[oT], join=(qb > 0))
                    deferred.append([3, fin])

            for i in range(len(pairs) + LOOK):
                if i < len(pairs):
                    emit_qk(i)
                if i >= LOOK:
                    emit_pv(i - LOOK)
                for dfr in list(deferred):
                    dfr[0] -= 1
                    if dfr[0] <= 0:
                        dfr[1]()
                        deferred.remove(dfr)
            for dfr in deferred:
                dfr[1]()
            deferred.clear()
            k.dma("sp", OT.ap[h * 64:(h + 1) * 64, :], oT[:, :], reads=[oT], writes=[])
        k.barrier()


def wo_phase(k, io, g, X_in, OT, X_out):
    gate = g.gates[1][0]
    with contextlib.ExitStack() as st:
        wo = k.sb([128, 8, 1024], BF16, stack=st, name="wo")
        for kc in range(8):
            k.dma("pool", wo[:, kc, :], io["wo"][kc * 128:(kc + 1) * 128, :], writes=[wo])
        xt_r = Rot([k.sb([128, 1024], F32, stack=st, name="wxt") for _ in range(4)])
        o_r = Rot([k.sb([128, 8, 128], BF16, stack=st, name="wo_o") for _ in range(4)])
        y_r = Rot([k.sb([128, 1024], F32, stack=st, name="wy") for _ in range(2)])
        WL = {}

        def wl(ti):
            xt = xt_r.next()
            k.dma("sp", xt[:, :], X_in.ap[ti * 128:(ti + 1) * 128, :], reads=[X_in], writes=[xt])
            o = o_r.next()
            k.dma("act", o[:, :, :], OT.ap[:, ti * 128:(ti + 1) * 128].rearrange("(c p) t -> p c t", p=128), writes=[o])
            WL[ti] = (xt, o)

        wl(0)
        wl(1)
        for ti in range(NT):
            if ti + 2 < NT:
                wl(ti + 2)
            xt, o = WL.pop(ti)
            y = y_r.next()
            for nb in range(2):
                py = g.psum.next()
                for c in range(8):
                    k.mm(py, py[:, :], o, o[:, c, :], wo, wo[:, c, nb * 512:(nb + 1) * 512], start=(c == 0), stop=(c == 7))
                k.op("dve", lambda e: e.tensor_tensor(out=y[:, nb * 512:(nb + 1) * 512], in0=py[:, :],
                                                      in1=gate[:, nb * 512:(nb + 1) * 512], op=ALU.mult),
                     reads=[py, gate], writes=[y], join=(nb > 0))
            k.op("pool", lambda e: e.tensor_tensor(out=y[:, :], in0=y[:, :], in1=xt[:, :], op=ALU.add), reads=[y, xt], writes=[y])
            k.dma("sp", X_out.ap[ti * 128:(ti + 1) * 128, :], y[:, :], reads=[y], writes=[])
        k.barrier()


def prep_inputs(inp, b):
    cf, cb = make_consts()
    f = lambda a: np.ascontiguousarray(a, dtype=np.float32)
    return dict(
        x=f(inp["x"][b]), c8=f(inp["c"][b].reshape(8, 128)), ada_w=f(inp["ada_w"].reshape(2048, 6144)), ada_b=f(inp["ada_b"]),
        nmg=f(inp["norm_mix_g"].reshape(16, 128)), nfg=f(inp["norm_ffn_g"].reshape(16, 128)),
        gw_in=f(inp["gmlp_w_in"][0]), gvg=f(inp["gmlp_v_g"].reshape(16, 128)), gvb=f(inp["gmlp_v_b"].reshape(1, 2048)),
        gws=f(inp["gmlp_w_s"][0]), gbs=f(inp["gmlp_b_s"].reshape(1, 1024)), gw_out=f(inp["gmlp_w_out"][0]),
        kvaw=f(inp["kv_ada_w"]), kvab=f(inp["kv_ada_b"].reshape(1, 2048)), kvng=f(inp["kv_norm_g"].reshape(8, 128)),
        kvw=f(inp["kv_w"]), kng=f(inp["k_norm_g"].reshape(1, 64)), fgb=f(inp["fgate_b"].reshape(1, 16)),
        wqg=f(inp["fox_w_qg"][0]), qng=f(inp["q_norm_g"].reshape(1, 64)), wo=f(inp["fox_w_o"][0]),
        rw=f(inp["router_w"].reshape(2048, 32)), rb=f(inp["router_b"]),
        w1=f(inp["exp_w1"].reshape(65536, 2048)), b1=f(inp["exp_b1"].reshape(64, 16, 128).transpose(0, 2, 1).reshape(64 * 128, 16)),
        w2=f(inp["exp_w2"].reshape(65536, 1024)), b2=f(inp["exp_b2"].reshape(64, 1024)),
        fing=f(inp["final_g"].reshape(1, 1024)), cf=cf, cb=cb,
    )


_NC_CACHE = {}


def build_program():
    nc = bass.Bass("TRN2", target_bir_lowering=False)
    with contextlib.ExitStack() as stack:
        k = K(nc, stack)
        io = declare_inputs(nc)
        g = G()
        setup(k, io, g)
        R = RState(k)
        FP = k.sb([128, NT, 16], F32, name="FP")
        X0 = Tk(io["x"], "x")
        X1 = k.dram("X1", [S, D], F32)
        X2 = k.dram("X2", [S, D], F32)
        X3 = k.dram("X3", [S, D], F32)
        OUT = k.dram("out", [S, D], F32, kind="ExternalOutput")
        BUF = k.dram("BUF", [32 * CAP, D], BF16)
        Y = k.dram("Y", [NB * 256, D], F32)
        KT = k.dram("KT", [D, S], BF16)
        QT = k.dram("QT", [D, S], BF16)
        GT = k.dram("GT", [D, S], BF16)
        V = k.dram("V", [S, D], BF16)
        OT = k.dram("OT", [D, S], BF16)
        FPT = k.dram("FPT", [16, S], F32)
        W1B = Tk(io["w1"], "w1")
        W2B = Tk(io["w2"], "w2")
        gmlp_phase(k, io, g, X0, X1)
        route_phase(k, io, g, 0, X1, BUF, R)
        expert_phase(k, io, g, 0, BUF, Y, R, W1B, W2B)
        combine_phase(k, io, g, 0, X1, Y, X2, R)
        kvq_phase(k, io, g, X2, KT, QT, GT, V, FP, FPT)
        attn_phase(k, io, g, KT, QT, GT, V, FP, FPT, OT)
        wo_phase(k, io, g, X2, OT, X3)
        route_phase(k, io, g, 1, X3, BUF, R)
        expert_phase(k, io, g, 1, BUF, Y, R, W1B, W2B)
        combine_phase(k, io, g, 1, X3, Y, OUT, R, final=True)
        k.barrier()
    return nc


def kernel(**inputs):
    inp = {kk: np.asarray(v) for kk, v in inputs.items()}
    if "nc" not in _NC_CACHE:
        _NC_CACHE["nc"] = build_program()
    nc = _NC_CACHE["nc"]
    shared = prep_inputs(inp, 0)
    in_maps = []
    for b in range(8):
        m = dict(shared)
        m["x"] = np.ascontiguousarray(inp["x"][b], dtype=np.float32)
        m["c8"] = np.ascontiguousarray(inp["c"][b].reshape(8, 128), dtype=np.float32)
        in_maps.append(m)
    res = run_bass_kernel_spmd(nc, in_maps, core_ids=list(range(8)))
    out = np.stack([np.asarray(r["out"], dtype=np.float32) for r in res.results], axis=0)
    return out
```
